# Optimizing a Trainium2 kernel written in Bass

```python
import jax
import jax.numpy as jnp
from jax import lax
import numpy as np

D_MODEL = 2048
BATCH = 16
SEQ = 2048
DEPTH = 1

D_FF = 5632
GLA_HEADS = 4
GLA_DK = 128
GLA_DV = 256
GLA_CHUNK = 64
GLA_GATE_RANK = 16
GLA_GATE_NORMALIZER = 16.0
MOBA_HEADS = 8
MOBA_HEAD_DIM = 128
MOBA_BLOCK = 256
MOBA_TOPK = 3
MOBA_QUERY_CHUNK = 16
ALIBI_MAX_BIAS = 8.0
NORM_EPS = 1e-6

GLA_QK_WIDTH = GLA_HEADS * GLA_DK
GLA_V_WIDTH = GLA_HEADS * GLA_DV
MOBA_WIDTH = MOBA_HEADS * MOBA_HEAD_DIM
MIX_WIDTH = GLA_V_WIDTH + MOBA_WIDTH
IN_SIZES = (GLA_QK_WIDTH, GLA_QK_WIDTH, GLA_V_WIDTH, GLA_V_WIDTH, GLA_GATE_RANK, MOBA_WIDTH, MOBA_WIDTH, MOBA_WIDTH)
IN_WIDTH = 2 * GLA_QK_WIDTH + 2 * GLA_V_WIDTH + GLA_GATE_RANK + 3 * MOBA_WIDTH

kernel_name = "hybrid_gla_moba_macaron_layer"


def rms_norm(x, w):
    xf = x.astype(jnp.float32)
    y = xf * lax.rsqrt(jnp.mean(xf * xf, axis=-1, keepdims=True) + NORM_EPS)
    return (y * w.astype(jnp.float32)).astype(x.dtype)


def swiglu(h, w_gate, w_up, w_down):
    return (jax.nn.silu(h @ w_gate) * (h @ w_up)) @ w_down


def alibi_slopes(n_heads):
    return jnp.exp2(-ALIBI_MAX_BIAS * jnp.arange(1, n_heads + 1, dtype=jnp.float32) / n_heads)


def gla_mixer(q, k, v, log_a, g, norm_w):
    B, S, H, dk = q.shape
    dv = v.shape[-1]
    C = GLA_CHUNK
    N = S // C

    def to_chunks(t):
        return t.reshape(B, N, C, H, t.shape[-1]).transpose(1, 0, 3, 2, 4)

    qc = to_chunks(q * (dk ** -0.5))
    kc = to_chunks(k)
    vc = to_chunks(v)
    Gc = jnp.cumsum(to_chunks(log_a.astype(jnp.float32)), axis=3)
    causal = jnp.tril(jnp.ones((C, C), dtype=bool))

    def step(state, inp):
        qn, kn, vn, Gn = inp
        inter = jnp.einsum('bhcd,bhde->bhce', qn * jnp.exp(Gn), state)
        diff = Gn[:, :, :, None, :] - Gn[:, :, None, :, :]
        decay = jnp.exp(jnp.where(causal[:, :, None], diff, -jnp.inf))
        scores = jnp.einsum('bhid,bhjd,bhijd->bhij', qn, kn, decay)
        intra = jnp.einsum('bhij,bhje->bhie', scores, vn)
        G_last = Gn[:, :, -1:, :]
        new_state = (jnp.exp(G_last[:, :, 0, :])[..., None] * state
                     + jnp.einsum('bhcd,bhce->bhde', kn * jnp.exp(G_last - Gn), vn))
        return new_state, inter + intra

    state0 = jnp.zeros((B, H, dk, dv), jnp.float32)
    _, o = lax.scan(step, state0, (qc, kc, vc, Gc))
    o = o.transpose(1, 0, 3, 2, 4).reshape(B, S, H, dv).astype(v.dtype)
    o = rms_norm(o, norm_w)
    return (o * jax.nn.silu(g)).reshape(B, S, H * dv)


def moba_mixer(q, k, v):
    B, S, H, hd = q.shape
    BS = MOBA_BLOCK
    QC = MOBA_QUERY_CHUNK
    NB = -(-S // BS)
    SP = NB * BS
    q = q.transpose(0, 2, 1, 3)
    k = k.transpose(0, 2, 1, 3)
    v = v.transpose(0, 2, 1, 3)
    pad = ((0, 0), (0, 0), (0, SP - S), (0, 0))
    kb = jnp.pad(k, pad).reshape(B, H, NB, BS, hd)
    vb = jnp.pad(v, pad).reshape(B, H, NB, BS, hd)

    qblk = jnp.arange(S) // BS
    kmean = jnp.mean(kb, axis=3)
    gate = jnp.einsum('bhsd,bhnd->bhsn', q, kmean).astype(jnp.float32)
    past = jnp.arange(NB)[None, :] < qblk[:, None]
    gate = jnp.where(past, gate, -jnp.inf)
    n_sel = min(MOBA_TOPK, NB)
    _, sel_idx = lax.top_k(gate, n_sel)
    sel_valid = sel_idx < qblk[:, None]

    slopes = alibi_slopes(H)
    scale = hd ** -0.5
    bi = jnp.arange(B)[:, None, None, None]
    hi = jnp.arange(H)[None, :, None, None]
    offs = jnp.arange(BS)

    def attend(c):
        start = c * QC
        q_c = lax.dynamic_slice_in_dim(q, start, QC, axis=2)
        idx_c = lax.dynamic_slice_in_dim(sel_idx, start, QC, axis=2)
        val_c = lax.dynamic_slice_in_dim(sel_valid, start, QC, axis=2)
        t = start + jnp.arange(QC)
        own = start // BS
        k_sel = kb[bi, hi, idx_c]
        v_sel = vb[bi, hi, idx_c]
        s_sel = idx_c[..., None] * BS + offs
        dist_sel = (t[:, None, None] - s_sel).astype(jnp.float32)
        l_sel = (jnp.einsum('bhqd,bhqnkd->bhqnk', q_c, k_sel).astype(jnp.float32) * scale
                 - slopes[None, :, None, None, None] * dist_sel)
        l_sel = jnp.where(val_c[..., None], l_sel, -jnp.inf)
        k_own = lax.dynamic_index_in_dim(kb, own, axis=2, keepdims=False)
        v_own = lax.dynamic_index_in_dim(vb, own, axis=2, keepdims=False)
        dist_own = t[:, None] - (own * BS + offs)[None, :]
        l_own = (jnp.einsum('bhqd,bhkd->bhqk', q_c, k_own).astype(jnp.float32) * scale
                 - slopes[None, :, None, None] * dist_own.astype(jnp.float32))
        l_own = jnp.where(dist_own >= 0, l_own, -jnp.inf)
        logits = jnp.concatenate([l_sel.reshape(B, H, QC, n_sel * BS), l_own], axis=-1)
        p = jax.nn.softmax(logits, axis=-1).astype(v.dtype)
        p_sel = p[..., :n_sel * BS].reshape(B, H, QC, n_sel, BS)
        p_own = p[..., n_sel * BS:]
        return (jnp.einsum('bhqnk,bhqnkd->bhqd', p_sel, v_sel)
                + jnp.einsum('bhqk,bhkd->bhqd', p_own, v_own))

    o = lax.map(attend, jnp.arange(S // QC))
    return o.transpose(1, 0, 3, 2, 4).reshape(B, S, H * hd)


def hybrid_mixer(h, w_in, w_decay_up, b_decay, gla_norm_w, w_out):
    B, S, _ = h.shape
    proj = h @ w_in
    points = []
    acc = 0
    for size in IN_SIZES[:-1]:
        acc += size
        points.append(acc)
    gq, gk, gv, gg, glr, mq, mk, mv = jnp.split(proj, points, axis=-1)
    log_a = jax.nn.log_sigmoid(glr @ w_decay_up + b_decay) / GLA_GATE_NORMALIZER
    gh = lambda t: t.reshape(B, S, GLA_HEADS, -1)
    mh = lambda t: t.reshape(B, S, MOBA_HEADS, -1)
    o_gla = gla_mixer(gh(gq), gh(gk), gh(gv), gh(log_a), gh(gg), gla_norm_w)
    o_moba = moba_mixer(mh(mq), mh(mk), mh(mv))
    return jnp.concatenate([o_gla, o_moba], axis=-1) @ w_out


def setup_inputs(seed: int = 0) -> dict:
    key = jax.random.key(seed)
    ks = jax.random.split(key, 18)
    L = DEPTH

    def normal(k, shape, fan_in):
        return jax.random.normal(k, shape, jnp.float32) * (fan_in ** -0.5)

    def gain(k, shape):
        return 1.0 + 0.05 * jax.random.normal(k, shape, jnp.float32)

    return {
        "x": jax.random.normal(ks[0], (BATCH, SEQ, D_MODEL), jnp.float32),
        "ffn1_pre_norm": gain(ks[1], (L, D_MODEL)),
        "ffn1_w_gate": normal(ks[2], (L, D_MODEL, D_FF), D_MODEL),
        "ffn1_w_up": normal(ks[3], (L, D_MODEL, D_FF), D_MODEL),
        "ffn1_w_down": normal(ks[4], (L, D_FF, D_MODEL), D_FF),
        "ffn1_post_norm": gain(ks[5], (L, D_MODEL)),
        "mix_pre_norm": gain(ks[6], (L, D_MODEL)),
        "w_in": normal(ks[7], (L, D_MODEL, IN_WIDTH), D_MODEL),
        "gla_w_decay_up": normal(ks[8], (L, GLA_GATE_RANK, GLA_QK_WIDTH), GLA_GATE_RANK),
        "gla_b_decay": 0.05 * jax.random.normal(ks[9], (L, GLA_QK_WIDTH), jnp.float32),
        "gla_out_norm": gain(ks[10], (L, GLA_DV)),
        "w_out": normal(ks[11], (L, MIX_WIDTH, D_MODEL), MIX_WIDTH),
        "mix_post_norm": gain(ks[12], (L, D_MODEL)),
        "ffn2_pre_norm": gain(ks[13], (L, D_MODEL)),
        "ffn2_w_gate": normal(ks[14], (L, D_MODEL, D_FF), D_MODEL),
        "ffn2_w_up": normal(ks[15], (L, D_MODEL, D_FF), D_MODEL),
        "ffn2_w_down": normal(ks[16], (L, D_FF, D_MODEL), D_FF),
        "ffn2_post_norm": gain(ks[17], (L, D_MODEL)),
    }


def reference(x, ffn1_pre_norm, ffn1_w_gate, ffn1_w_up, ffn1_w_down, ffn1_post_norm,
              mix_pre_norm, w_in, gla_w_decay_up, gla_b_decay, gla_out_norm, w_out, mix_post_norm,
              ffn2_pre_norm, ffn2_w_gate, ffn2_w_up, ffn2_w_down, ffn2_post_norm):
    h = x
    for l in range(DEPTH):
        f1 = swiglu(rms_norm(h, ffn1_pre_norm[l]), ffn1_w_gate[l], ffn1_w_up[l], ffn1_w_down[l])
        h = h + 0.5 * rms_norm(f1, ffn1_post_norm[l])
        m = hybrid_mixer(rms_norm(h, mix_pre_norm[l]), w_in[l], gla_w_decay_up[l],
                         gla_b_decay[l], gla_out_norm[l], w_out[l])
        h = h + rms_norm(m, mix_post_norm[l])
        f2 = swiglu(rms_norm(h, ffn2_pre_norm[l]), ffn2_w_gate[l], ffn2_w_up[l], ffn2_w_down[l])
        h = h + 0.5 * rms_norm(f2, ffn2_post_norm[l])
    return h
```

```python
import bisect
import contextlib
import numpy as np
import concourse.bass as bass
import concourse.mybir as mybir
from concourse.bass_utils import run_bass_kernel_spmd

F32 = mybir.dt.float32
BF16 = mybir.dt.bfloat16
AF = mybir.ActivationFunctionType
ALU = mybir.AluOpType
AX = mybir.AxisListType

D = 2048
F = 5632
KD = D // 128
KF = F // 128
SEQ = 2048
NSEQ = 2
INW = 6160
EPS = 1e-6
NCORES = 8

C_GQ, C_GK, C_GV, C_GG, C_LR, C_MQ, C_MK, C_MV = 0, 512, 1024, 2048, 3072, 3088, 4112, 5136


class Sch:
    def __init__(self, nc):
        self.nc = nc
        self.ops = []
        self.lastw = {}
        self.readers = {}

    def add(self, eng, fn, r=(), w=(), dma=None):
        i = len(self.ops)
        deps = set()
        for k in r:
            if k in self.lastw:
                deps.add(self.lastw[k])
        for k in w:
            if k in self.lastw:
                deps.add(self.lastw[k])
            for j in self.readers.get(k, ()):
                deps.add(j)
        self.ops.append(dict(eng=eng, fn=fn, deps=deps, dma=dma))
        for k in r:
            self.readers.setdefault(k, []).append(i)
        for k in w:
            self.lastw[k] = i
            self.readers[k] = []
        return i

    def barrier(self):
        n = len(self.ops)
        last = {}
        for i, o in enumerate(self.ops):
            if o['fn'] is not None and not o['dma']:
                last[o['eng']] = i
        alld = set(last.values()) | {i for i, o in enumerate(self.ops) if o['dma']}
        for e in ['sp', 'act', 'dve', 'pool', 'pe']:
            self.ops.append(dict(eng=e, fn=None, deps=set(alld), dma=None, bar=True))
        self.lastw = {}
        self.readers = {}

    def plan(self, final_wait_eng='sp'):
        ops = self.ops
        engs = ['sp', 'act', 'dve', 'pool', 'pe']
        dma_sems = sorted({o['dma'] for o in ops if o['dma']})
        dma_idx = {s: [i for i, o in enumerate(ops) if o['dma'] == s] for s in dma_sems}
        milestone = [False] * len(ops)
        for i, o in enumerate(ops):
            for d in o['deps']:
                od = ops[d]
                if od['dma']:
                    continue
                if od['eng'] == o['eng'] and o['eng'] == 'pe' and not o['dma'] and not o.get('bar'):
                    continue
                if od['eng'] == o['eng'] and o.get('bar'):
                    continue
                milestone[d] = True
        ms_index = {}
        cnt = {e: 0 for e in engs}
        for i, o in enumerate(ops):
            if o['dma'] or o['fn'] is None:
                continue
            if milestone[i]:
                cnt[o['eng']] += 1
                ms_index[i] = cnt[o['eng']]
        plan = {e: [] for e in engs}
        waited = {e: {} for e in engs}
        for i, o in enumerate(ops):
            engname = o['eng']
            need = {}
            for d in o['deps']:
                od = ops[d]
                if od['dma']:
                    s = od['dma']
                    v = 16 * bisect.bisect_left(dma_idx[s], i)
                    key = ('d', s)
                else:
                    if od['eng'] == engname and engname == 'pe' and not o['dma']:
                        continue
                    if od['eng'] == engname and o.get('bar'):
                        continue
                    key = ('e', od['eng'])
                    v = ms_index[d]
                if v > need.get(key, 0):
                    need[key] = v
            waits = []
            for key, v in need.items():
                if waited[engname].get(key, 0) >= v:
                    continue
                waited[engname][key] = v
                waits.append((key, v))
            inc = None
            if o['fn'] is not None:
                if o['dma']:
                    inc = (('d', o['dma']), 16)
                elif milestone[i]:
                    inc = (('e', engname), 1)
            plan[engname].append((waits, i, inc))
        fin = [(('d', s), 16 * len(dma_idx[s])) for s in dma_sems]
        fin += [(('e', en), cnt[en]) for en in engs if cnt[en] > 0 and en != final_wait_eng]
        plan[final_wait_eng].append((fin, None, None))
        return plan, dma_sems

    def simulate(self):
        plan, _ = self.plan()
        sem = {}
        pos = {e: 0 for e in plan}
        progress = True
        while progress:
            progress = False
            for e, lst in plan.items():
                while pos[e] < len(lst):
                    waits, i, inc = lst[pos[e]]
                    if all(sem.get(k, 0) >= v for k, v in waits):
                        if inc:
                            sem[inc[0]] = sem.get(inc[0], 0) + inc[1]
                        pos[e] += 1
                        progress = True
                    else:
                        break
        stuck = {e: (pos[e], len(lst)) for e, lst in plan.items() if pos[e] < len(lst)}
        if stuck:
            for e in stuck:
                waits, i, inc = plan[e][pos[e]]
                print("STUCK", e, pos[e], len(plan[e]), [(k, v, sem.get(k, 0)) for k, v in waits])
        return not stuck, max(sem.values()) if sem else 0

    def emit(self, final_wait_eng='sp'):
        nc = self.nc
        ops = self.ops
        engs = ['sp', 'act', 'dve', 'pool', 'pe']
        plan, dma_sems = self.plan(final_wait_eng)
        with contextlib.ExitStack() as st:
            esem = {e: st.enter_context(nc.semaphore('s_' + e)) for e in engs}
            dsem = {s: st.enter_context(nc.semaphore('d_' + s)) for s in dma_sems}
            getsem = lambda key: dsem[key[1]] if key[0] == 'd' else esem[key[1]]
            block = st.enter_context(nc.Block())

            def make(engname):
                def body(e):
                    for waits, i, inc in plan[engname]:
                        for key, v in waits:
                            e.wait_ge(getsem(key), v)
                        if i is None or ops[i]['fn'] is None:
                            continue
                        ins = ops[i]['fn'](e)
                        if inc:
                            ins.then_inc(getsem(inc[0]), inc[1])
                return body

            block.sync(make('sp'))
            block.scalar(make('act'))
            block.vector(make('dve'))
            block.gpsimd(make('pool'))
            block.tensor(make('pe'))


class Arena:
    BASE = 16512
    LIMIT = 229344

    def __init__(self, nc):
        self.nc = nc
        self.cur = self.BASE
        self.n = 0
        self.mark_ = self.BASE

    def t(self, name, shape, dtype):
        esz = 4 if dtype == F32 else 2
        nbytes = esz
        for s in shape[1:]:
            nbytes *= s
        self.cur = (self.cur + 63) // 64 * 64
        off = self.cur
        self.cur += nbytes
        assert self.cur <= self.LIMIT, (name, self.cur - self.BASE)
        self.n += 1
        return self.nc.alloc_sbuf_tensor_at("%s_%d" % (name, self.n), list(shape), dtype, offset=off)

    def mark(self):
        self.mark_ = self.cur

    def reset(self):
        self.cur = self.mark_


def build_program(ntok=4096, phases=("A", "B", "C"), dbg=False):
    nc = bass.Bass("TRN2", target_bir_lowering=False)
    dt = lambda n, s, d=F32, k="ExternalInput": nc.dram_tensor(n, list(s), d, kind=k).ap()
    x = dt("x", [ntok, D])
    w_f32 = {}
    for pfx in ("ffn1", "ffn2"):
        w_f32[pfx + "_w_gate"] = dt(pfx + "_w_gate", [D, F])
        w_f32[pfx + "_w_up"] = dt(pfx + "_w_up", [D, F])
        w_f32[pfx + "_w_down"] = dt(pfx + "_w_down", [F, D])
    w_f32["w_in"] = dt("w_in", [D, INW])
    w_f32["w_out"] = dt("w_out", [D, D])
    gains = {n: dt(n, [1, D]) for n in ("ffn1_pre_norm", "ffn1_post_norm", "mix_pre_norm", "mix_post_norm",
                                         "ffn2_pre_norm", "ffn2_post_norm")}
    wdu = dt("gla_w_decay_up", [16, 512])
    bdec = dt("gla_b_decay", [1, 512])
    gno = dt("gla_out_norm", [1, 256])
    cst = dt("consts", [128, 128 + NCB])
    ensd = dt("ens", [8, 1024])
    KTd = dt("KTd", [8, 128, ntok], BF16, "Internal")
    Vd = dt("Vd", [ntok, 1024], BF16, "Internal")
    out = dt("out", [ntok, D], F32, "ExternalOutput")
    wb = {n: dt(n + "_b", ap.shape, BF16, "Internal") for n, ap in w_f32.items()}
    h1 = dt("h1", [ntok, D], F32, "Internal")
    h2 = dt("h2", [ntok, D], F32, "Internal")

    S = Sch(nc)
    A = Arena(nc)
    psf = [nc.alloc_psum_tensor("psf%d" % i, [128, 512], F32) for i in range(6)]
    pst = [nc.alloc_psum_tensor("pst%d" % i, [128, 1024], BF16) for i in range(2)]

    cf = A.t("cf", [128, 128], F32)
    identb = A.t("identb", [128, 128], BF16)
    S.add('sp', lambda e: e.dma_start(out=cf[:], in_=cst[:, 0:128]), w=['cf'], dma='cst')
    S.add('dve', lambda e: e.tensor_copy(out=identb[:], in_=cf[:, 0:128]), r=['cf'], w=['identb'])
    epsc = A.t("epsc", [128, 1], F32)
    S.add('pool', lambda e: e.memset(epsc[:], EPS), w=['epsc'])
    onec = A.t("onec", [128, 1], F32)
    S.add('pool', lambda e: e.memset(onec[:], 1.0), w=['onec'])
    A.mark()

    def cast_ffn_fg(pfx, fg, rot=False):
        g, u, d = pfx + "_w_gate", pfx + "_w_up", pfx + "_w_down"
        c0, c1 = fg * 512, (fg + 1) * 512
        sfx = ("_%d" % (fg % 4)) if rot else ""
        tok = (lambda n: [('ctok', n, fg % 4)]) if rot else (lambda n: [])
        S.add('pool', lambda e: e.dma_start(out=wb[g][:, c0:c1], in_=w_f32[g][:, c0:c1]), w=[('wb', g, fg)] + tok(g), dma='cast_' + g + sfx)
        S.add('pool', lambda e: e.dma_start(out=wb[u][:, c0:c1], in_=w_f32[u][:, c0:c1]), w=[('wb', u, fg)] + tok(u), dma='cast_' + u + sfx)
        S.add('pool', lambda e: e.dma_start(out=wb[d][c0:c1, :], in_=w_f32[d][c0:c1, :]), w=[('wb', d, fg)] + tok(d), dma='cast_' + d + sfx)

    def cast_rows_j(n, j):
        src, dst = w_f32[n], wb[n]
        step = src.shape[0] // 4
        a, b = j * step, (j + 1) * step
        S.add('pool', lambda e: e.dma_start(out=dst[a:b, :], in_=src[a:b, :]), w=[('wb', n, j)], dma='cast_' + n)

    later_casts = []
    if "B" in phases:
        later_casts += [(lambda n=n, j=j: cast_rows_j(n, j)) for n in ("w_in", "w_out") for j in range(4)]
    if "C" in phases:
        later_casts += [(lambda fg=fg: cast_ffn_fg("ffn2", fg)) for fg in range(KF // 4)]
    if "A" not in phases:
        for f_ in later_casts:
            f_()
        later_casts = []

    def wkeys(n):
        return [('wb', n, j) for j in range(4)]

    pbank = [0]

    def nb():
        b = pbank[0]
        pbank[0] = (b + 1) % 6
        return b

    def ffn_phase(tag, src, dst, n_g, n_u, n_d, g_pre_d, g_post_d):
        A.reset()
        ntiles = ntok // 512
        big = A.t("big", [128, 4, D], F32)
        xres = [A.t("xres", [128, D], F32) for _ in range(2)]
        xnT = A.t("xnT", [128, KD, 512], BF16)
        actT = [A.t("actT", [128, 4, 512], BF16) for _ in range(2)]
        wg = [A.t("wg", [128, KD, 512], BF16) for _ in range(2)]
        wu = [A.t("wu", [128, KD, 512], BF16) for _ in range(2)]
        wd = [A.t("wd", [128, 4, D], BF16) for _ in range(2)]
        gpre = A.t("gpre", [128, D], F32)
        gpost = A.t("gpost", [128, D], F32)
        xn_tm = [A.t("xn_tm", [128, D], BF16) for _ in range(2)]
        sgt = [A.t("sgt", [128, 512], F32) for _ in range(2)]
        ss = A.t("ss", [128, 8], F32)
        rstd = A.t("rstd", [128, 8], F32)
        K = lambda *a: (tag,) + a
        wgb, wub, wdb = wb[n_g], wb[n_u], wb[n_d]

        S.add('sp', lambda e: e.dma_start(out=gpre[:], in_=g_pre_d.partition_broadcast(128)), w=[K('gpre')], dma=tag + 'g')
        S.add('sp', lambda e: e.dma_start(out=gpost[:], in_=g_post_d.partition_broadcast(128)), w=[K('gpost')], dma=tag + 'g')

        NG = KF // 4
        steps = [(t, fg) for t in range(ntiles) for fg in range(NG)]
        xr_cnt = [0]

        def load_gu(i):
            t, fg = steps[i]
            sl = i % 2
            S.add('sp', lambda e: e.dma_start(out=wg[sl][:], in_=wgb[:, fg * 512:(fg + 1) * 512].rearrange("(k p) c -> p k c", p=128)),
                  r=[('wb', n_g, fg)], w=[K('wg', sl), ('ctok', n_g, fg % 4)], dma=tag + 'wg%d' % sl)
            S.add('sp', lambda e: e.dma_start(out=wu[sl][:], in_=wub[:, fg * 512:(fg + 1) * 512].rearrange("(k p) c -> p k c", p=128)),
                  r=[('wb', n_u, fg)], w=[K('wu', sl), ('ctok', n_u, fg % 4)], dma=tag + 'wu%d' % sl)

        def load_d(i):
            t, fg = steps[i]
            sl = i % 2
            S.add('sp', lambda e: e.dma_start(out=wd[sl][:], in_=wdb[fg * 512:(fg + 1) * 512, :].rearrange("(c p) d -> p c d", p=128)),
                  r=[('wb', n_d, fg)], w=[K('wd', sl), ('ctok', n_d, fg % 4)], dma=tag + 'wd%d' % sl)

        def norm_stats(srcs, col0, n):
            S.add('pool', lambda e: e.memset(ss[:, col0:col0 + n], 0.0), w=[K('ss', col0)])
            for j, (ap, keys, junk, jkeys) in enumerate(srcs):
                S.add('act', lambda e, ap=ap, junk=junk, j=j: e.activation(out=junk, in_=ap, func=AF.Square,
                                                                            accum_out=ss[:, col0 + j:col0 + j + 1]),
                      r=list(keys) + [K('ss', col0)], w=list(jkeys) + [K('ss', col0)])

            S.add('act', lambda e: e.activation(out=rstd[:, col0:col0 + n], in_=ss[:, col0:col0 + n], func=AF.Sqrt,
                                                bias=epsc[:, 0:1], scale=1.0 / D),
                  r=[K('ss', col0), 'epsc'], w=[K('rstd', col0)])
            S.add('dve', lambda e: e.reciprocal(out=rstd[:, col0:col0 + n], in_=rstd[:, col0:col0 + n]),
                  r=[K('rstd', col0)], w=[K('rstd', col0)])

        def prologue(t):
            for s in range(4):
                sl = xr_cnt[0] % 2
                xr_cnt[0] += 1
                r0 = t * 512 + s * 128
                S.add('pool', lambda e, sl=sl, r0=r0: e.dma_start(out=xres[sl][:], in_=src[r0:r0 + 128, :]),
                      r=[('dram', tag + 'src', r0)], w=[K('xres', sl)], dma=tag + 'xr%d' % sl)
                m = s % 2
                norm_stats([(xres[sl][:], [K('xres', sl)], xn_tm[m][:], [K('xn_tm', m)])], s, 1)
                S.add('dve', lambda e, sl=sl, m=m, s=s: e.scalar_tensor_tensor(
                    out=xn_tm[m][:], in0=xres[sl][:], scalar=rstd[:, s:s + 1], in1=gpre[:], op0=ALU.mult, op1=ALU.mult),
                    r=[K('xres', sl), K('rstd', s), K('gpre')], w=[K('xn_tm', m)])
                for hf in range(2):
                    tb = (2 * s + hf) % 2

                    def tr(e, m=m, hf=hf, tb=tb):
                        for j in range(8):
                            k = hf * 8 + j
                            ins = e.transpose(pst[tb][:, j * 128:(j + 1) * 128], xn_tm[m][:, k * 128:(k + 1) * 128], identb[:])
                        return ins
                    S.add('pe', tr, r=[K('xn_tm', m), 'identb'], w=[('pst', tb)])
                    S.add('dve', lambda e, hf=hf, tb=tb, s=s: e.tensor_copy(
                        out=xnT[:, hf * 8:(hf + 1) * 8, s * 128:(s + 1) * 128],
                        in_=pst[tb][:].rearrange("p (k c) -> p k c", c=128)),
                        r=[('pst', tb)], w=[K('xnT', s)])

        def gu(i):
            t, fg = steps[i]
            sl = i % 2
            for c in range(4):
                bg, bu = nb(), nb()

                def mm(e, c=c, bg=bg, bu=bu):
                    for k in range(KD):
                        e.matmul(psf[bg][:], lhsT=wg[sl][:, k, c * 128:(c + 1) * 128], rhs=xnT[:, k, :], start=(k == 0), stop=(k == KD - 1))
                    for k in range(KD):
                        ins = e.matmul(psf[bu][:], lhsT=wu[sl][:, k, c * 128:(c + 1) * 128], rhs=xnT[:, k, :], start=(k == 0), stop=(k == KD - 1))
                    return ins
                S.add('pe', mm, r=[K('wg', sl), K('wu', sl)] + [K('xnT', s) for s in range(4)], w=[('psf', bg), ('psf', bu)])
                p = c % 2
                S.add('act', lambda e, bg=bg, p=p: e.activation(out=sgt[p][:], in_=psf[bg][:], func=AF.Silu),
                      r=[('psf', bg)], w=[K('sgt', p)])
                S.add('dve', lambda e, bu=bu, p=p, c=c: e.tensor_tensor(out=actT[sl][:, c, :], in0=psf[bu][:], in1=sgt[p][:], op=ALU.mult),
                      r=[('psf', bu), K('sgt', p)], w=[K('actT', sl, c)])

        import os as _os2
        _dd = _os2.environ.get("DBG_DOWN", "")

        def down(i):
            t, fg = steps[i]
            sl = i % 2
            for dg in range(4):
                for s in range(4):
                    b = nb()

                    def mm(e, b=b, dg=dg, s=s):
                        for c in range(4):
                            ins = e.matmul(psf[b][:], lhsT=actT[sl][:, c, s * 128:(s + 1) * 128],
                                           rhs=wd[sl][:, c, dg * 512:(dg + 1) * 512], start=(c == 0), stop=(c == 3))
                        return ins
                    if _dd == "dma":
                        continue
                    S.add('pe', mm, r=[K('wd', sl)] + [K('actT', sl, c) for c in range(4)], w=[('psf', b)])
                    if _dd == "mm":
                        continue
                    dstap = big[:, s, dg * 512:(dg + 1) * 512]
                    if fg == 0:
                        S.add('dve', lambda e, b=b, dstap=dstap: e.tensor_copy(out=dstap, in_=psf[b][:]),
                              r=[('psf', b)], w=[K('big', s, dg)])
                    else:
                        S.add('dve', lambda e, b=b, dstap=dstap: e.tensor_tensor(out=dstap, in0=psf[b][:], in1=dstap, op=ALU.add),
                              r=[('psf', b), K('big', s, dg)], w=[K('big', s, dg)])

        def epilogue(t):
            for s in range(4):
                sl = xr_cnt[0] % 2
                xr_cnt[0] += 1
                r0 = t * 512 + s * 128
                S.add('pool', lambda e, sl=sl, r0=r0: e.dma_start(out=xres[sl][:], in_=src[r0:r0 + 128, :]),
                      r=[('dram', tag + 'src', r0)], w=[K('xres', sl)], dma=tag + 'xr%d' % sl)
                m = s % 2
                bkeys = [K('big', s, dg) for dg in range(4)]
                norm_stats([(big[:, s, :], bkeys, xn_tm[m][:], [K('xn_tm', m)])], 4 + s, 1)
                S.add('dve', lambda e, s=s: e.scalar_tensor_tensor(
                    out=big[:, s, :], in0=big[:, s, :], scalar=rstd[:, 4 + s:5 + s], in1=gpost[:], op0=ALU.mult, op1=ALU.mult),
                    r=bkeys + [K('rstd', 4 + s), K('gpost')], w=bkeys)
                S.add('dve', lambda e, s=s, sl=sl: e.scalar_tensor_tensor(
                    out=xres[sl][:], in0=big[:, s, :], scalar=0.5, in1=xres[sl][:], op0=ALU.mult, op1=ALU.add),
                    r=bkeys + [K('xres', sl)], w=[K('xres', sl)])
                S.add('pool', lambda e, sl=sl, r0=r0: e.dma_start(out=dst[r0:r0 + 128, :], in_=xres[sl][:]),
                      r=[K('xres', sl)], w=[('dram', tag + 'dst', r0)], dma=tag + 'xr%d' % sl)

        N = len(steps)
        own_cast = (tag == "A")
        if own_cast:
            cast_ffn_fg("ffn1", 0, True)
            cast_ffn_fg("ffn1", 1, True)
            cast_ffn_fg("ffn1", 2, True)
        import os as _os
        _stop = _os.environ.get("DBG_STOP", "")
        if _stop == "pro":
            prologue(0)
            return
        if _stop == "gu":
            load_gu(0)
            prologue(0)
            gu(0)
            return
        if _stop == "gud":
            load_gu(0)
            load_d(0)
            prologue(0)
            gu(0)
            down(0)
            return
        load_gu(0)
        load_d(0)
        if N > 1:
            load_gu(1)
        prologue(0)
        gu(0)
        for i in range(N):
            if own_cast:
                if i + 3 < NG:
                    cast_ffn_fg("ffn1", i + 3, True)
                elif later_casts:
                    later_casts.pop(0)()
            if i + 1 < N:
                load_d(i + 1)
            if i + 2 < N:
                load_gu(i + 2)
            if i + 1 < N:
                if steps[i + 1][1] == 0:
                    prologue(steps[i + 1][0])
                gu(i + 1)
            down(i)
            if steps[i][1] == NG - 1:
                epilogue(steps[i][0])
        if own_cast:
            while later_casts:
                later_casts.pop(0)()

    def mixer_phase(src, dst):
        A.reset()
        tag = "B"
        K = lambda *a: (tag,) + a
        NTB = ntok // 256
        winb, woutb = wb["w_in"], wb["w_out"]
        cB = A.t("cB", [128, 1664], F32)
        c2cur = A.t("c2cur", [128, 384], F32)
        S.add('sp', lambda e: e.dma_start(out=cB[:], in_=cst[:, 128:128 + 1664]), w=['cB'], dma='Bc')
        Tf, Uf, T4 = cB[:, 0:128], cB[:, 128:256], cB[:, 256:768]
        distc = [cB[:, 768:1024], cB[:, 1024:1280], cB[:, 1280:1536]]
        biasT = lambda h, m: cB[:, 1536 + h * 16 + m:1536 + h * 16 + m + 1]
        c2 = lambda qb, j: c2cur[:, j * 128:(j + 1) * 128]
        ens = A.t("ens", [8, 8, 128], BF16)
        onesb = A.t("onesb", [128, 128], BF16)
        S.add('pool', lambda e: e.memset(onesb[:], 1.0), w=['onesb'])
        gpre = A.t("gpre", [128, D], F32)
        gpost = A.t("gpost", [128, D], F32)
        bdecb = A.t("bdecb", [128, 512], F32)
        gnob = A.t("gnob", [128, 256], F32)
        wdus = A.t("wdus", [16, 512], F32)
        S.add('sp', lambda e: e.dma_start(out=gpre[:], in_=gains["mix_pre_norm"].partition_broadcast(128)), w=[K('gpre')], dma='Bg')
        S.add('sp', lambda e: e.dma_start(out=gpost[:], in_=gains["mix_post_norm"].partition_broadcast(128)), w=[K('gpost')], dma='Bg')
        S.add('sp', lambda e: e.dma_start(out=bdecb[:], in_=bdec.partition_broadcast(128)), w=['bdecb'], dma='Bg')
        S.add('sp', lambda e: e.dma_start(out=gnob[:], in_=gno.partition_broadcast(128)), w=['gnob'], dma='Bg')
        S.add('sp', lambda e: e.dma_start(out=wdus[:], in_=wdu), w=['wdus'], dma='Bg')
        hb = A.t("hb", [128, 2, D], F32)
        xn_tm = [A.t("xn_tm", [128, D], BF16) for _ in range(2)]
        xnT = A.t("xnT", [128, KD, 256], BF16)
        wst = [A.t("wst", [128, KD, 512], BF16) for _ in range(2)]
        glrT = A.t("glrT", [16, 256], F32)
        lae = A.t("lae", [128, D], F32)
        la = lae[:, 0:1024].rearrange("p (s c) -> p s c", c=512)
        eR = lae[:, 1024:2048].rearrange("p (s c) -> p s c", c=512)
        junk = A.t("junk", [128, D], BF16)
        e1T = A.t("e1T", [128, 4, 256], F32)
        e2T = A.t("e2T", [128, 4, 256], F32)
        qtT = A.t("qtT", [128, 4, 256], BF16)
        ktT = A.t("ktT", [128, 4, 256], BF16)
        khat = A.t("khat", [128, 2, 512], BF16)
        vt = A.t("vt", [128, 2, 1024], BF16)
        sg = A.t("sg", [128, 2, 1024], F32)
        Sst = A.t("Sst", [128, 4, 256], F32)
        Sb = A.t("Sb", [128, 4, 256], BF16)
        QT = A.t("QT", [128, 8, 256], BF16)
        KTt = A.t("KTt", [128, 8, 256], BF16)
        Vt = A.t("Vt", [128, 2, 1024], BF16)
        kmT = A.t("kmT", [128, 8, 8], F32)
        kmTb = A.t("kmTb", [128, 8, 8], BF16)
        KTp = [A.t("KTp", [128, 1792], BF16) for _ in range(2)]
        Vp = [A.t("Vp", [128, 14, 128], BF16) for _ in range(2)]
        gm = A.t("gm", [128, 128], F32)
        mx8 = A.t("mx8", [128, 16, 8], F32)
        sel = A.t("sel", [128, 128], F32)
        mbb = A.t("mbb", [128, 128], BF16)
        MBT = A.t("MBT", [8, 8, 256], BF16)
        ptile = [A.t("ptile", [128, 256], BF16) for _ in range(3)]
        ltmp = [A.t("ltmp", [128, 256], F32) for _ in range(2)]
        rec = [A.t("rec", [128, 256], F32) for _ in range(2)]
        mixT = A.t("mixT", [128, 16, 256], BF16)
        AT = [A.t("AT", [128, 4, 128], BF16) for _ in range(2)]
        ogb = A.t("ogb", [128, 1024], BF16)
        mst = A.t("mst", [128, D], F32)
        ogt = mst[:, 0:1024]
        ensf = mst[0:8, 0:1024]
        S.add('sp', lambda e: e.dma_start(out=ensf, in_=ensd), w=[K('mst', 0), K('mst', 1)], dma='Bc')
        S.add('dve', lambda e: e.tensor_copy(out=ens[:].rearrange("p n c -> p (n c)"), in_=ensf), r=[K('mst', 0), K('mst', 1)], w=['ens'])
        ss = A.t("ss", [128, 16], F32)
        rstd = A.t("rstd", [128, 16], F32)
        S.add('pool', lambda e: e.memset(kmT[:], 0.0), w=['kmT'])
        slopes = [2.0 ** (-(h + 1)) for h in range(8)]
        wcnt = [0]

        def rstd_of(col, n, scale):
            S.add('act', lambda e: e.activation(out=rstd[:, col:col + n], in_=ss[:, col:col + n], func=AF.Sqrt,
                                                bias=epsc[:, 0:1], scale=scale), r=[K('ss', col), 'epsc'], w=[K('rstd', col)])
            S.add('dve', lambda e: e.reciprocal(out=rstd[:, col:col + n], in_=rstd[:, col:col + n]),
                  r=[K('rstd', col)], w=[K('rstd', col)])

        def wload(srcb, c0, ncol):
            sl = wcnt[0] % 2
            wcnt[0] += 1
            S.add('sp', lambda e: e.dma_start(out=wst[sl][:, :, 0:ncol], in_=srcb[:, c0:c0 + ncol].rearrange("(k p) c -> p k c", p=128)),
                  r=wkeys("w_in") + wkeys("w_out"), w=[K('wst', sl)], dma='Bw%d' % sl)
            return sl

        def fm_mm(sl, c0, M, evac, bank=None):
            b = nb() if bank is None else bank

            def mm(e):
                for k in range(KD):
                    ins = e.matmul(psf[b][0:M, 0:256], lhsT=wst[sl][:, k, c0:c0 + M], rhs=xnT[:, k, :], start=(k == 0), stop=(k == KD - 1))
                return ins
            S.add('pe', mm, r=[K('wst', sl), K('xnT', 0), K('xnT', 1)], w=[('psf', b)])
            evac(b)

        def tm_mm(sl, s, evac):
            b = nb()

            def mm(e):
                for k in range(KD):
                    ins = e.matmul(psf[b][:], lhsT=xnT[:, k, s * 128:(s + 1) * 128], rhs=wst[sl][:, k, :], start=(k == 0), stop=(k == KD - 1))
                return ins
            S.add('pe', mm, r=[K('wst', sl), K('xnT', s)], w=[('psf', b)])
            evac(b)

        LAEK = [K('la', 0), K('la', 1), K('eR', 0), K('eR', 1)]

        def pro1(tbn, s):
            r0 = tbn * 256 + s * 128
            S.add('sp', lambda e: e.dma_start(out=lae[:], in_=src[r0:r0 + 128, :]), r=[('dram', 'Bsrc', tbn)], w=LAEK, dma='Bx')
            S.add('pool', lambda e: e.memset(ss[:, s:s + 1], 0.0), w=[K('ss', s)])
            S.add('act', lambda e: e.activation(out=junk[:], in_=lae[:], func=AF.Square, accum_out=ss[:, s:s + 1]),
                  r=LAEK + [K('ss', s)], w=[K('junk'), K('ss', s)])
            rstd_of(s, 1, 1.0 / D)
            S.add('dve', lambda e: e.scalar_tensor_tensor(out=xn_tm[s][:], in0=lae[:], scalar=rstd[:, s:s + 1], in1=gpre[:],
                                                          op0=ALU.mult, op1=ALU.mult),
                  r=LAEK + [K('rstd', s), K('gpre')], w=[K('xn_tm', s)])

        def pro2(s):
            for hf in range(2):
                def tr(e, hf=hf):
                    for j in range(8):
                        k = hf * 8 + j
                        ins = e.transpose(pst[hf][:, j * 128:(j + 1) * 128], xn_tm[s][:, k * 128:(k + 1) * 128], identb[:])
                    return ins
                S.add('pe', tr, r=[K('xn_tm', s), 'identb'], w=[('pst', hf)])
                S.add('dve', lambda e, hf=hf: e.tensor_copy(out=xnT[:, hf * 8:(hf + 1) * 8, s * 128:(s + 1) * 128],
                                                            in_=pst[hf][:].rearrange("p (k c) -> p k c", c=128)),
                      r=[('pst', hf)], w=[K('xnT', s)])

        def do_tile(tb):
            tok0 = tb * 256
            qb = tb % 8
            seq0 = (tb // 8) * SEQ
            if qb == 0:
                S.add('pool', lambda e: e.memset(Sst[:], 0.0), w=[K('Sst', h) for h in range(4)])
                S.add('pool', lambda e: e.memset(Sb[:], 0.0), w=[K('Sb')])
            S.add('sp', lambda e: e.dma_start(out=c2cur[:], in_=cst[:, 128 + 1664 + qb * 384:128 + 1664 + (qb + 1) * 384]), w=['c2cur'], dma='Bc2')
            S.add('pool', lambda e: e.dma_start(out=hb[:], in_=src[tok0:tok0 + 256, :].rearrange("(s p) d -> p s d", p=128)),
                  r=[('dram', 'Bsrc', tb)], w=[K('hb', 0), K('hb', 1)], dma='Bh')
            if tb == 0:
                pro1(0, 0)
                pro2(0)
                pro1(0, 1)
                pro2(1)
            sl = wload(winb, C_LR, 16)
            fm_mm(sl, 0, 16, lambda b: S.add('dve', lambda e: e.tensor_copy(out=glrT[:], in_=psf[b][0:16, 0:256]),
                                              r=[('psf', b)], w=['glrT']))
            for s in range(2):
                b = nb()
                S.add('pe', lambda e, b=b, s=s: e.matmul(psf[b][:], lhsT=glrT[:, s * 128:(s + 1) * 128], rhs=wdus[:], start=True, stop=True),
                      r=['glrT', 'wdus'], w=[('psf', b)])
                S.add('dve', lambda e, b=b, s=s: e.tensor_tensor(out=la[:, s, :], in0=psf[b][:], in1=bdecb[:], op=ALU.add),
                      r=[('psf', b), 'bdecb'], w=[K('la', s)])
                S.add('act', lambda e, s=s: e.activation(out=la[:, s, :], in_=la[:, s, :], func=AF.Exp, scale=-1.0), r=[K('la', s)], w=[K('la', s)])
                S.add('act', lambda e, s=s: e.activation(out=la[:, s, :], in_=la[:, s, :], func=AF.Ln, bias=onec[:, 0:1]), r=[K('la', s), 'onec'], w=[K('la', s)])
            for j in range(2):
                sl = wload(winb, C_GV + j * 512, 512)
                for s in range(2):
                    tm_mm(sl, s, lambda b, s=s, j=j: S.add('dve', lambda e: e.tensor_copy(out=vt[:, s, j * 512:(j + 1) * 512], in_=psf[b][:]),
                                                           r=[('psf', b)], w=[K('vt', s, j)]))
            for j in range(2):
                sl = wload(winb, C_GG + j * 512, 512)
                for s in range(2):
                    tm_mm(sl, s, lambda b, s=s, j=j: S.add('act', lambda e: e.activation(out=sg[:, s, j * 512:(j + 1) * 512], in_=psf[b][:], func=AF.Silu),
                                                           r=[('psf', b)], w=[K('sg', s, j)]))
            for j in range(2):
                sl = wload(winb, C_MQ + j * 512, 512)
                for hh in range(4):
                    h = j * 4 + hh
                    fm_mm(sl, hh * 128, 128, lambda b, h=h: S.add('dve', lambda e: e.tensor_scalar(
                        out=QT[:, h, :], in0=psf[b][:, 0:256], scalar1=128.0 ** -0.5, scalar2=None, op0=ALU.mult),
                        r=[('psf', b)], w=[K('QT', h)]))
            for j in range(2):
                sl = wload(winb, C_MK + j * 512, 512)
                for hh in range(4):
                    h = j * 4 + hh

                    def ev(b, h=h):
                        S.add('dve', lambda e: e.tensor_copy(out=KTt[:, h, :], in_=psf[b][:, 0:256]), r=[('psf', b)], w=[K('KTt', h)])
                        S.add('dve', lambda e: e.tensor_reduce(out=kmT[:, h, qb:qb + 1], in_=psf[b][:, 0:256], axis=AX.X, op=ALU.add),
                              r=[('psf', b)], w=['kmT'])
                    fm_mm(sl, hh * 128, 128, ev)
            for j in range(2):
                sl = wload(winb, C_MV + j * 512, 512)
                for s in range(2):
                    tm_mm(sl, s, lambda b, s=s, j=j: S.add('dve', lambda e: e.tensor_copy(out=Vt[:, s, j * 512:(j + 1) * 512], in_=psf[b][:]),
                                                           r=[('psf', b)], w=[K('Vt', s, j)]))
            for s in range(2):
                b = nb()

                def cum(e, b=b, s=s):
                    for h in range(4):
                        ins = e.matmul(psf[b][:, h * 128:(h + 1) * 128], lhsT=la[:, s, h * 128:(h + 1) * 128], rhs=Tf, start=True, stop=True)
                    return ins
                S.add('pe', cum, r=[K('la', s), 'cB'], w=[('psf', b)])
                S.add('act', lambda e, b=b, s=s: e.activation(out=e1T[:, :, s * 128:(s + 1) * 128], in_=psf[b][:].rearrange("p (h c) -> p h c", c=128),
                                                              func=AF.Exp, scale=-1.0 / 16.0), r=[('psf', b)], w=[K('e1T', s)])
                S.add('act', lambda e, b=b, s=s: e.activation(out=e2T[:, :, s * 128:(s + 1) * 128], in_=psf[b][:].rearrange("p (h c) -> p h c", c=128),
                                                              func=AF.Exp, scale=1.0 / 16.0), r=[('psf', b)], w=[K('e2T', s)])
                b = nb()
                S.add('pe', lambda e, b=b, s=s: e.matmul(psf[b][:], lhsT=Uf, rhs=la[:, s, :], start=True, stop=True),
                      r=[K('la', s), 'cB'], w=[('psf', b)])
                S.add('act', lambda e, b=b, s=s: e.activation(out=eR[:, s, :], in_=psf[b][:], func=AF.Exp, scale=-1.0 / 16.0),
                      r=[('psf', b)], w=[K('eR', s)])
            E12 = [K('e1T', 0), K('e1T', 1)]
            E22 = [K('e2T', 0), K('e2T', 1)]
            sl = wload(winb, C_GQ, 512)
            for h in range(4):
                fm_mm(sl, h * 128, 128, lambda b, h=h: S.add('dve', lambda e: e.scalar_tensor_tensor(
                    out=qtT[:, h, :], in0=psf[b][:, 0:256], scalar=128.0 ** -0.5, in1=e1T[:, h, :], op0=ALU.mult, op1=ALU.mult),
                    r=[('psf', b)] + E12, w=[K('qtT', h)]))
            sl = wload(winb, C_GK, 512)
            for h in range(4):
                fm_mm(sl, h * 128, 128, lambda b, h=h: S.add('dve', lambda e: e.tensor_tensor(
                    out=ktT[:, h, :], in0=psf[b][:, 0:256], in1=e2T[:, h, :], op=ALU.mult), r=[('psf', b)] + E22, w=[K('ktT', h)]))
            for s in range(2):
                tm_mm(sl, s, lambda b, s=s: S.add('dve', lambda e: e.tensor_tensor(out=khat[:, s, :], in0=psf[b][:], in1=eR[:, s, :], op=ALU.mult),
                                                  r=[('psf', b), K('eR', s)], w=[K('khat', s)]))
            KTtk = [K('KTt', h) for h in range(8)]
            Vtk = [K('Vt', s, j) for s in range(2) for j in range(2)]
            if qb < 7:
                S.add('pool', lambda e, tok0=tok0: e.dma_start(out=KTd[:, :, tok0:tok0 + 256].rearrange("h d t -> d h t"), in_=KTt[:]),
                      r=KTtk, w=[('dram', 'KTd', tb)], dma='Bkc')
                S.add('pool', lambda e, tok0=tok0: e.dma_start(out=Vd[tok0:tok0 + 256, :].rearrange("(s p) c -> p s c", p=128), in_=Vt[:]),
                      r=Vtk, w=[('dram', 'Vd', tb)], dma='Bkc')
            S.add('dve', lambda e: e.tensor_copy(out=kmTb[:], in_=kmT[:]), r=['kmT'], w=['kmTb'])

            bgt = nb()

            def gmm(e, bgt=bgt):
                for s in range(2):
                    for h in range(8):
                        g = s * 8 + h
                        ins = e.matmul(psf[bgt][:, g * 8:(g + 1) * 8], lhsT=QT[:, h, s * 128:(s + 1) * 128], rhs=kmTb[:, h, :], start=True, stop=True)
                return ins
            S.add('pe', gmm, r=[K('QT', h) for h in range(8)] + ['kmTb'], w=[('psf', bgt)])
            S.add('dve', lambda e, bgt=bgt, qb=qb: e.tensor_tensor(out=gm[:], in0=psf[bgt][:, 0:128], in1=c2(qb, 0), op=ALU.add),
                  r=[('psf', bgt), 'c2cur'], w=['gm'])

            def selop1(e):
                for g in range(16):
                    ins = e.max(out=mx8[:, g, :], in_=gm[:, g * 8:(g + 1) * 8])
                return ins
            S.add('dve', selop1, r=['gm'], w=['mx8'])

            def selop2(e):
                for g in range(16):
                    ins = e.tensor_scalar(out=sel[:, g * 8:(g + 1) * 8], in0=gm[:, g * 8:(g + 1) * 8], scalar1=mx8[:, g, 2:3], scalar2=None, op0=ALU.is_ge)
                return ins
            S.add('dve', selop2, r=['gm', 'mx8'], w=['sel'])
            S.add('dve', lambda e, qb=qb: e.tensor_tensor(out=sel[:], in0=sel[:], in1=c2(qb, 1), op=ALU.mult), r=['sel', 'c2cur'], w=['sel'])
            S.add('dve', lambda e, qb=qb: e.scalar_tensor_tensor(out=mbb[:], in0=sel[:], scalar=32768.0, in1=c2(qb, 2), op0=ALU.mult, op1=ALU.add),
                  r=['sel', 'c2cur'], w=['mbb'])

            def trm(e):
                for s in range(2):
                    for h in range(8):
                        g = s * 8 + h
                        ins = e.transpose(pst[h // 4][0:8, (h % 4) * 256 + s * 128:(h % 4) * 256 + (s + 1) * 128], mbb[:, g * 8:(g + 1) * 8], identb[:])
                return ins
            S.add('pe', trm, r=['mbb', 'identb'], w=[('pst', 0), ('pst', 1)])
            for q in range(2):
                S.add('dve', lambda e, q=q: e.tensor_copy(out=MBT[:, q * 4:(q + 1) * 4, :].rearrange("p h c -> p (h c)"), in_=pst[q][0:8, :]),
                      r=[('pst', q)], w=[K('MBT', q)])

            if tb + 1 < NTB:
                pro1(tb + 1, 0)
            CS = [slice(0, 128), slice(128, 256)]
            bsc, bds, bos = {}, {}, {}
            for s in range(2):
                cs = CS[s]
                b = nb()
                bsc[s] = b

                def sc(e, b=b, cs=cs):
                    for h in range(4):
                        ins = e.matmul(psf[b][:, h * 128:(h + 1) * 128], lhsT=ktT[:, h, cs], rhs=qtT[:, h, cs], start=True, stop=True)
                    return ins
                S.add('pe', sc, r=[K('ktT', h) for h in range(4)] + [K('qtT', h) for h in range(4)], w=[('psf', b)])
            for s in range(2):
                bd = [nb(), nb()]
                bds[s] = bd

                def dsm(e, bd=bd, s=s):
                    for h in range(4):
                        ins = e.matmul(psf[bd[h // 2]][:, (h % 2) * 256:(h % 2 + 1) * 256], lhsT=khat[:, s, h * 128:(h + 1) * 128],
                                       rhs=vt[:, s, h * 256:(h + 1) * 256], start=True, stop=True)
                    return ins
                S.add('pe', dsm, r=[K('khat', s), K('vt', s, 0), K('vt', s, 1)], w=[('psf', bd[0]), ('psf', bd[1])])
            for s in range(2):
                S.add('dve', lambda e, s=s: e.tensor_tensor(out=AT[s][:].rearrange("p h c -> p (h c)"), in0=psf[bsc[s]][:], in1=T4, op=ALU.mult),
                      r=[('psf', bsc[s]), 'cB'], w=[K('AT', s)])
            for s in range(2):
                cs = CS[s]
                bo = [nb(), nb()]
                bd = bds[s]

                def om(e, bo=bo, cs=cs, s=s):
                    for h in range(4):
                        o_ap = psf[bo[h // 2]][:, (h % 2) * 256:(h % 2 + 1) * 256]
                        e.matmul(o_ap, lhsT=AT[s][:, h, :], rhs=vt[:, s, h * 256:(h + 1) * 256], start=True, stop=False)
                        ins = e.matmul(o_ap, lhsT=qtT[:, h, cs], rhs=Sb[:, h, :], start=False, stop=True)
                    return ins
                S.add('pe', om, r=[K('AT', s), K('vt', s, 0), K('vt', s, 1), K('Sb')] + [K('qtT', h) for h in range(4)],
                      w=[('psf', bo[0]), ('psf', bo[1])])
                for h in range(4):
                    S.add('dve', lambda e, h=h, bd=bd, s=s: e.scalar_tensor_tensor(
                        out=Sst[:, h, :], in0=Sst[:, h, :], scalar=e1T[:, h, s * 128 + 127:s * 128 + 128],
                        in1=psf[bd[h // 2]][:, (h % 2) * 256:(h % 2 + 1) * 256], op0=ALU.mult, op1=ALU.add),
                        r=[K('Sst', h), K('e1T', s), ('psf', bd[h // 2])], w=[K('Sst', h)])
                S.add('pool', lambda e: e.tensor_copy(out=Sb[:], in_=Sst[:]), r=[K('Sst', h) for h in range(4)], w=[K('Sb')])
                S.add('pool', lambda e: e.memset(ss[:, 4:8], 0.0), w=[K('ss', 4)])
                for h in range(4):
                    S.add('act', lambda e, h=h, bo=bo: e.activation(out=junk[:, h * 256:(h + 1) * 256],
                                                                    in_=psf[bo[h // 2]][:, (h % 2) * 256:(h % 2 + 1) * 256],
                                                                    func=AF.Square, accum_out=ss[:, 4 + h:5 + h]),
                          r=[('psf', bo[h // 2]), K('ss', 4)], w=[K('junk'), K('ss', 4)])
                rstd_of(4, 4, 1.0 / 256.0)
                for h in range(4):
                    S.add('dve', lambda e, h=h, bo=bo: e.scalar_tensor_tensor(
                        out=mst[:, h * 256:(h + 1) * 256], in0=psf[bo[h // 2]][:, (h % 2) * 256:(h % 2 + 1) * 256],
                        scalar=rstd[:, 4 + h:5 + h], in1=gnob[:], op0=ALU.mult, op1=ALU.mult),
                        r=[('psf', bo[h // 2]), K('rstd', 4), 'gnob'], w=[K('mst', h // 2)])
                S.add('dve', lambda e, s=s: e.tensor_tensor(out=ogb[:], in0=mst[:, 0:1024], in1=sg[:, s, :], op=ALU.mult),
                      r=[K('mst', 0), K('mst', 1), K('sg', s, 0), K('sg', s, 1)], w=[K('ogb')])

                def trg(e):
                    for c in range(8):
                        ins = e.transpose(pst[0][:, c * 128:(c + 1) * 128], ogb[:, c * 128:(c + 1) * 128], identb[:])
                    return ins
                S.add('pe', trg, r=[K('ogb'), 'identb'], w=[('pst', 0)])
                S.add('dve', lambda e, cs=cs: e.tensor_copy(out=mixT[:, 0:8, cs], in_=pst[0][:].rearrange("p (k c) -> p k c", c=128)),
                      r=[('pst', 0)], w=[K('mixT', 'g', cs.start)])

            LAG = 2
            nkt = 2 * (qb + 1)
            items = [(h, kt) for h in range(8) for kt in range(nkt)]
            info = {}

            def emit_loads(h):
                hp = h % 2
                if qb > 0:
                    S.add('pool', lambda e: e.dma_start(out=KTp[hp][:, 0:qb * 256], in_=KTd[h, :, seq0:seq0 + qb * 256]),
                          r=[('dram', 'KTd', tb - 1 - i) for i in range(qb)], w=[K('KTp', hp)], dma='Bkp%d' % hp)
                    S.add('pool', lambda e: e.dma_start(out=Vp[hp][:, 0:2 * qb, :],
                                                        in_=Vd[seq0:seq0 + qb * 256, h * 128:(h + 1) * 128].rearrange("(k p) d -> p k d", p=128)),
                          r=[('dram', 'Vd', tb - 1 - i) for i in range(qb)], w=[K('Vp', hp)], dma='Bvp%d' % hp)

            def emit_s(idx):
                h, kt = items[idx]
                hp = h % 2
                n = kt // 2
                if n == qb:
                    ko = kt - 2 * qb
                    kT_ap = KTt[:, h, ko * 128:(ko + 1) * 128]
                    v_ap = Vt[:, ko, h * 128:(h + 1) * 128]
                    rk, rv = [K('KTt', h)], [K('Vt', ko, h // 4)]
                    dc, m = distc[1 + ko], 0
                else:
                    kT_ap = KTp[hp][:, kt * 128:(kt + 1) * 128]
                    v_ap = Vp[hp][:, kt, :]
                    rk, rv = [K('KTp', hp)], [K('Vp', hp)]
                    dc, m = distc[0], 2 * qb - kt
                bS, pt = idx % 2, idx % 3
                info[idx] = (v_ap, rv, pt)

                def smm(e):
                    e.matmul(psf[bS][:, 0:256], lhsT=kT_ap, rhs=QT[:, h, :], start=True, stop=False)
                    return e.matmul(psf[bS][:, 0:256], lhsT=ens[:, n, :], rhs=MBT[:, h, :], start=False, stop=True)
                S.add('pe', smm, r=rk + [K('QT', h), K('MBT', h // 4), 'ens'], w=[('psf', bS)])
                S.add('dve', lambda e: e.scalar_tensor_tensor(out=ltmp[bS][:], in0=dc, scalar=-slopes[h], in1=psf[bS][:, 0:256],
                                                              op0=ALU.mult, op1=ALU.add),
                      r=[('psf', bS), 'cB'], w=[K('ltmp', bS)])
                S.add('act', lambda e: e.activation(out=ptile[pt][:], in_=ltmp[bS][:], func=AF.Exp, bias=biasT(h, m)),
                      r=[K('ltmp', bS), 'cB'], w=[K('ptile', pt)])

            def emit_pv(idx):
                h, kt = items[idx]
                hp = h % 2
                bO, bL = 2 + 2 * hp, 3 + 2 * hp
                v_ap, rv, pt = info[idx]

                def pv(e):
                    e.matmul(psf[bO][:, 0:256], lhsT=v_ap, rhs=ptile[pt][:], start=(kt == 0), stop=(kt == nkt - 1))
                    return e.matmul(psf[bL][:, 0:256], lhsT=onesb[:], rhs=ptile[pt][:], start=(kt == 0), stop=(kt == nkt - 1))
                S.add('pe', pv, r=rv + [K('ptile', pt), 'onesb'], w=[('psf', bO), ('psf', bL)])
                if kt == nkt - 1:
                    S.add('dve', lambda e: e.reciprocal(out=rec[hp][:], in_=psf[bL][:, 0:256]), r=[('psf', bL)], w=[K('rec', hp)])
                    S.add('dve', lambda e: e.tensor_tensor(out=mixT[:, 8 + h, :], in0=psf[bO][:, 0:256], in1=rec[hp][:], op=ALU.mult),
                          r=[('psf', bO), K('rec', hp)], w=[K('mixT', 'm', h)])

            emit_loads(0)
            emit_loads(1)
            for idx in range(len(items)):
                h, kt = items[idx]
                if kt == 0 and h >= 1 and h + 1 < 8:
                    emit_loads(h + 1)
                if kt == 0 and tb + 1 < NTB:
                    if h == 2:
                        pro2(0)
                        pro1(tb + 1, 1)
                    if h == 5:
                        pro2(1)
                emit_s(idx)
                if idx >= LAG:
                    emit_pv(idx - LAG)
            for idx in range(max(0, len(items) - LAG), len(items)):
                emit_pv(idx)
            pbank[0] = 0
            mixk = [K('mixT', 'g', 0), K('mixT', 'g', 128)] + [K('mixT', 'm', h) for h in range(8)]
            MS = [mst, lae]
            MK = [[K('mst', j) for j in range(4)], LAEK]
            for j in range(4):
                sl = wload(woutb, j * 512, 512)
                for s in range(2):
                    b = nb()

                    def mmo(e, b=b, s=s, sl=sl):
                        for k in range(KD):
                            ins = e.matmul(psf[b][:], lhsT=mixT[:, k, s * 128:(s + 1) * 128], rhs=wst[sl][:, k, :], start=(k == 0), stop=(k == KD - 1))
                        return ins
                    S.add('pe', mmo, r=mixk + [K('wst', sl)], w=[('psf', b)])
                    S.add('dve', lambda e, b=b, j=j, s=s: e.tensor_copy(out=MS[s][:, j * 512:(j + 1) * 512], in_=psf[b][:]),
                          r=[('psf', b)], w=([K('mst', j)] if s == 0 else LAEK))
            for s in range(2):
                mk = MK[s]
                S.add('pool', lambda e, s=s: e.memset(ss[:, 8 + s:9 + s], 0.0), w=[K('ss', 8 + s)])
                S.add('act', lambda e, s=s: e.activation(out=junk[:], in_=MS[s][:], func=AF.Square, accum_out=ss[:, 8 + s:9 + s]),
                      r=mk + [K('ss', 8 + s)], w=[K('junk'), K('ss', 8 + s)])
                rstd_of(8 + s, 1, 1.0 / D)
                S.add('dve', lambda e, s=s: e.scalar_tensor_tensor(out=MS[s][:], in0=MS[s][:], scalar=rstd[:, 8 + s:9 + s], in1=gpost[:],
                                                                   op0=ALU.mult, op1=ALU.mult), r=mk + [K('rstd', 8 + s), K('gpost')], w=mk)
                S.add('dve', lambda e, s=s: e.tensor_tensor(out=hb[:, s, :], in0=MS[s][:], in1=hb[:, s, :], op=ALU.add),
                      r=mk + [K('hb', s)], w=[K('hb', s)])
                S.add('pool', lambda e, s=s: e.dma_start(out=dst[tok0 + s * 128:tok0 + (s + 1) * 128, :], in_=hb[:, s, :]),
                      r=[K('hb', s)], w=[('dram', 'Bdst', tb, s)], dma='Bo')

        for tb_ in range(NTB):
            do_tile(tb_)

    if "A" in phases:
        ffn_phase("A", x, h1 if ("B" in phases or "C" in phases) else out, "ffn1_w_gate", "ffn1_w_up", "ffn1_w_down",
                  gains["ffn1_pre_norm"], gains["ffn1_post_norm"])
        S.barrier()
    if "B" in phases:
        mixer_phase(h1 if "A" in phases else x, h2 if "C" in phases else out)
        S.barrier()
    if "C" in phases:
        srcC = h2 if "B" in phases else (h1 if "A" in phases else x)
        ffn_phase("C", srcC, out, "ffn2_w_gate", "ffn2_w_up", "ffn2_w_down",
                  gains["ffn2_pre_norm"], gains["ffn2_post_norm"])
    ok, mx = S.simulate()
    assert ok, 'sync deadlock'
    S.emit()
    return nc


NCB = 1792 + 3072


def make_consts():
    c = np.zeros((128, 128 + NCB), np.float32)
    c[:, 0:128] = np.eye(128, dtype=np.float32)
    b = c[:, 128:]
    j = np.arange(128)[:, None]
    i = np.arange(128)[None, :]
    T = (j <= i).astype(np.float32)
    b[:, 0:128] = T
    b[:, 128:256] = (j > i).astype(np.float32)
    b[:, 256:768] = np.tile(T, (1, 4))
    t = np.arange(256)[None, :]
    sl = np.arange(128)[:, None]
    b[:, 768:1024] = (t - sl).astype(np.float32)
    for kt in range(2):
        dd = (t - (kt * 128 + sl)).astype(np.float32)
        b[:, 1024 + kt * 256:1280 + kt * 256] = np.where(dd >= 0, dd, 1e6)
    slopes = 2.0 ** (-8.0 * np.arange(1, 9) / 8.0)
    for h in range(8):
        for m in range(16):
            b[:, 1536 + h * 16 + m] = -slopes[h] * m * 128.0
    for qb in range(8):
        n = np.arange(8)
        pastneg = np.where(n < qb, 0.0, -1e30).astype(np.float32)
        past01 = (n < qb).astype(np.float32)
        c3 = ((n == qb).astype(np.float32) - 1.0) * 32768.0
        base = 1664 + qb * 384
        b[:, base:base + 128] = np.tile(pastneg, 16)[None, :]
        b[:, base + 128:base + 256] = np.tile(past01, 16)[None, :]
        b[:, base + 256:base + 384] = np.tile(c3, 16)[None, :]
    return c


def make_ens():
    e = np.zeros((8, 1024), np.float32)
    for n in range(8):
        e[n, n * 128:(n + 1) * 128] = 1.0
    return e


_NC_CACHE = {}


def kernel(**inputs):
    x = np.ascontiguousarray(inputs["x"], dtype=np.float32)
    B = x.shape[0]
    per = B // NCORES
    ntok = per * SEQ
    key = ntok
    if key not in _NC_CACHE:
        _NC_CACHE[key] = build_program(ntok)
    nc = _NC_CACHE[key]
    shared = {}
    for n in ("ffn1_w_gate", "ffn1_w_up", "ffn1_w_down", "ffn2_w_gate", "ffn2_w_up", "ffn2_w_down", "w_in", "w_out"):
        shared[n] = np.ascontiguousarray(inputs[n][0], dtype=np.float32)
    for n in ("ffn1_pre_norm", "ffn1_post_norm", "mix_pre_norm", "mix_post_norm", "ffn2_pre_norm", "ffn2_post_norm",
              "gla_b_decay", "gla_out_norm"):
        shared[n] = np.ascontiguousarray(inputs[n], dtype=np.float32).reshape(1, -1)
    shared["gla_w_decay_up"] = np.ascontiguousarray(inputs["gla_w_decay_up"][0], dtype=np.float32)
    shared["consts"] = make_consts()
    shared["ens"] = make_ens()
    in_maps = []
    for c in range(NCORES):
        m = dict(shared)
        m["x"] = x[c * per:(c + 1) * per].reshape(ntok, D)
        in_maps.append(m)
    res = run_bass_kernel_spmd(nc, in_maps, core_ids=list(range(NCORES)))
    outs = [np.asarray(r["out"]).reshape(per, SEQ, D) for r in res.results]
    return np.concatenate(outs, axis=0).astype(np.float32)
```

```python
import bisect
import contextlib
import numpy as np
import concourse.bass as bass
import concourse.mybir as mybir
from concourse.bass_utils import run_bass_kernel_spmd

F32 = mybir.dt.float32
BF16 = mybir.dt.bfloat16
AF = mybir.ActivationFunctionType
ALU = mybir.AluOpType
AX = mybir.AxisListType

D = 2048
F = 5632
KD = D // 128
KF = F // 128
SEQ = 2048
NSEQ = 2
INW = 6160
EPS = 1e-6
NCORES = 8

C_GQ, C_GK, C_GV, C_GG, C_LR, C_MQ, C_MK, C_MV = 0, 512, 1024, 2048, 3072, 3088, 4112, 5136


class Sch:
    def __init__(self, nc):
        self.nc = nc
        self.ops = []
        self.lastw = {}
        self.readers = {}

    def add(self, eng, fn, r=(), w=(), dma=None):
        i = len(self.ops)
        deps = set()
        for k in r:
            if k in self.lastw:
                deps.add(self.lastw[k])
        for k in w:
            if k in self.lastw:
                deps.add(self.lastw[k])
            for j in self.readers.get(k, ()):
                deps.add(j)
        self.ops.append(dict(eng=eng, fn=fn, deps=deps, dma=dma))
        for k in r:
            self.readers.setdefault(k, []).append(i)
        for k in w:
            self.lastw[k] = i
            self.readers[k] = []
        return i

    def barrier(self):
        n = len(self.ops)
        last = {}
        for i, o in enumerate(self.ops):
            if o['fn'] is not None and not o['dma']:
                last[o['eng']] = i
        alld = set(last.values()) | {i for i, o in enumerate(self.ops) if o['dma']}
        for e in ['sp', 'act', 'dve', 'pool', 'pe']:
            self.ops.append(dict(eng=e, fn=None, deps=set(alld), dma=None, bar=True))
        self.lastw = {}
        self.readers = {}

    def plan(self, final_wait_eng='sp'):
        ops = self.ops
        engs = ['sp', 'act', 'dve', 'pool', 'pe']
        dma_sems = sorted({o['dma'] for o in ops if o['dma']})
        dma_idx = {s: [i for i, o in enumerate(ops) if o['dma'] == s] for s in dma_sems}
        milestone = [False] * len(ops)
        for i, o in enumerate(ops):
            for d in o['deps']:
                od = ops[d]
                if od['dma']:
                    continue
                if od['eng'] == o['eng'] and o['eng'] == 'pe' and not o['dma'] and not o.get('bar'):
                    continue
                if od['eng'] == o['eng'] and o.get('bar'):
                    continue
                milestone[d] = True
        ms_index = {}
        cnt = {e: 0 for e in engs}
        for i, o in enumerate(ops):
            if o['dma'] or o['fn'] is None:
                continue
            if milestone[i]:
                cnt[o['eng']] += 1
                ms_index[i] = cnt[o['eng']]
        plan = {e: [] for e in engs}
        waited = {e: {} for e in engs}
        for i, o in enumerate(ops):
            engname = o['eng']
            need = {}
            for d in o['deps']:
                od = ops[d]
                if od['dma']:
                    s = od['dma']
                    v = 16 * bisect.bisect_left(dma_idx[s], i)
                    key = ('d', s)
                else:
                    if od['eng'] == engname and engname == 'pe' and not o['dma']:
                        continue
                    if od['eng'] == engname and o.get('bar'):
                        continue
                    key = ('e', od['eng'])
                    v = ms_index[d]
                if v > need.get(key, 0):
                    need[key] = v
            waits = []
            for key, v in need.items():
                if waited[engname].get(key, 0) >= v:
                    continue
                waited[engname][key] = v
                waits.append((key, v))
            inc = None
            if o['fn'] is not None:
                if o['dma']:
                    inc = (('d', o['dma']), 16)
                elif milestone[i]:
                    inc = (('e', engname), 1)
            plan[engname].append((waits, i, inc))
        fin = [(('d', s), 16 * len(dma_idx[s])) for s in dma_sems]
        fin += [(('e', en), cnt[en]) for en in engs if cnt[en] > 0 and en != final_wait_eng]
        plan[final_wait_eng].append((fin, None, None))
        return plan, dma_sems

    def simulate(self):
        plan, _ = self.plan()
        sem = {}
        pos = {e: 0 for e in plan}
        progress = True
        while progress:
            progress = False
            for e, lst in plan.items():
                while pos[e] < len(lst):
                    waits, i, inc = lst[pos[e]]
                    if all(sem.get(k, 0) >= v for k, v in waits):
                        if inc:
                            sem[inc[0]] = sem.get(inc[0], 0) + inc[1]
                        pos[e] += 1
                        progress = True
                    else:
                        break
        stuck = {e: (pos[e], len(lst)) for e, lst in plan.items() if pos[e] < len(lst)}
        if stuck:
            for e in stuck:
                waits, i, inc = plan[e][pos[e]]
                print("STUCK", e, pos[e], len(plan[e]), [(k, v, sem.get(k, 0)) for k, v in waits])
        return not stuck, max(sem.values()) if sem else 0

    def emit(self, final_wait_eng='sp'):
        nc = self.nc
        ops = self.ops
        engs = ['sp', 'act', 'dve', 'pool', 'pe']
        plan, dma_sems = self.plan(final_wait_eng)
        with contextlib.ExitStack() as st:
            esem = {e: st.enter_context(nc.semaphore('s_' + e)) for e in engs}
            dsem = {s: st.enter_context(nc.semaphore('d_' + s)) for s in dma_sems}
            getsem = lambda key: dsem[key[1]] if key[0] == 'd' else esem[key[1]]
            block = st.enter_context(nc.Block())

            def make(engname):
                def body(e):
                    for waits, i, inc in plan[engname]:
                        for key, v in waits:
                            e.wait_ge(getsem(key), v)
                        if i is None or ops[i]['fn'] is None:
                            continue
                        ins = ops[i]['fn'](e)
                        if inc:
                            ins.then_inc(getsem(inc[0]), inc[1])
                return body

            block.sync(make('sp'))
            block.scalar(make('act'))
            block.vector(make('dve'))
            block.gpsimd(make('pool'))
            block.tensor(make('pe'))


class Arena:
    BASE = 16512
    LIMIT = 229344

    def __init__(self, nc):
        self.nc = nc
        self.cur = self.BASE
        self.n = 0
        self.mark_ = self.BASE

    def t(self, name, shape, dtype):
        esz = 4 if dtype == F32 else 2
        nbytes = esz
        for s in shape[1:]:
            nbytes *= s
        self.cur = (self.cur + 63) // 64 * 64
        off = self.cur
        self.cur += nbytes
        assert self.cur <= self.LIMIT, (name, self.cur - self.BASE)
        self.n += 1
        return self.nc.alloc_sbuf_tensor_at("%s_%d" % (name, self.n), list(shape), dtype, offset=off)

    def mark(self):
        self.mark_ = self.cur

    def reset(self):
        self.cur = self.mark_


def build_program(ntok=4096, phases=("A", "B", "C"), dbg=False):
    nc = bass.Bass("TRN2", target_bir_lowering=False)
    dt = lambda n, s, d=F32, k="ExternalInput": nc.dram_tensor(n, list(s), d, kind=k).ap()
    x = dt("x", [ntok, D])
    w_f32 = {}
    for pfx in ("ffn1", "ffn2"):
        w_f32[pfx + "_w_gate"] = dt(pfx + "_w_gate", [D, F])
        w_f32[pfx + "_w_up"] = dt(pfx + "_w_up", [D, F])
        w_f32[pfx + "_w_down"] = dt(pfx + "_w_down", [F, D])
    w_f32["w_in"] = dt("w_in", [D, INW])
    w_f32["w_out"] = dt("w_out", [D, D])
    gains = {n: dt(n, [1, D]) for n in ("ffn1_pre_norm", "ffn1_post_norm", "mix_pre_norm", "mix_post_norm",
                                         "ffn2_pre_norm", "ffn2_post_norm")}
    wdu = dt("gla_w_decay_up", [16, 512])
    bdec = dt("gla_b_decay", [1, 512])
    gno = dt("gla_out_norm", [1, 256])
    cst = dt("consts", [128, 128 + NCB])
    ensd = dt("ens", [8, 1024])
    KTd = dt("KTd", [8, 128, ntok], BF16, "Internal")
    Vd = dt("Vd", [ntok, 1024], BF16, "Internal")
    out = dt("out", [ntok, D], F32, "ExternalOutput")
    wb = {n: dt(n + "_b", ap.shape, BF16, "Internal") for n, ap in w_f32.items()}
    h1 = dt("h1", [ntok, D], F32, "Internal")
    h2 = dt("h2", [ntok, D], F32, "Internal")

    S = Sch(nc)
    A = Arena(nc)
    psf = [nc.alloc_psum_tensor("psf%d" % i, [128, 512], F32) for i in range(6)]
    pst = [nc.alloc_psum_tensor("pst%d" % i, [128, 1024], BF16) for i in range(2)]

    cf = A.t("cf", [128, 128], F32)
    identb = A.t("identb", [128, 128], BF16)
    S.add('sp', lambda e: e.dma_start(out=cf[:], in_=cst[:, 0:128]), w=['cf'], dma='cst')
    S.add('dve', lambda e: e.tensor_copy(out=identb[:], in_=cf[:, 0:128]), r=['cf'], w=['identb'])
    epsc = A.t("epsc", [128, 1], F32)
    S.add('pool', lambda e: e.memset(epsc[:], EPS), w=['epsc'])
    onec = A.t("onec", [128, 1], F32)
    S.add('pool', lambda e: e.memset(onec[:], 1.0), w=['onec'])
    A.mark()

    def cast_ffn_fg(pfx, fg, rot=False):
        g, u, d = pfx + "_w_gate", pfx + "_w_up", pfx + "_w_down"
        c0, c1 = fg * 512, (fg + 1) * 512
        sfx = ("_%d" % (fg % 4)) if rot else ""
        tok = (lambda n: [('ctok', n, fg % 4)]) if rot else (lambda n: [])
        S.add('pool', lambda e: e.dma_start(out=wb[g][:, c0:c1], in_=w_f32[g][:, c0:c1]), w=[('wb', g, fg)] + tok(g), dma='cast_' + g + sfx)
        S.add('pool', lambda e: e.dma_start(out=wb[u][:, c0:c1], in_=w_f32[u][:, c0:c1]), w=[('wb', u, fg)] + tok(u), dma='cast_' + u + sfx)
        S.add('pool', lambda e: e.dma_start(out=wb[d][c0:c1, :], in_=w_f32[d][c0:c1, :]), w=[('wb', d, fg)] + tok(d), dma='cast_' + d + sfx)

    def cast_rows_j(n, j):
        src, dst = w_f32[n], wb[n]
        step = src.shape[0] // 4
        a, b = j * step, (j + 1) * step
        S.add('pool', lambda e: e.dma_start(out=dst[a:b, :], in_=src[a:b, :]), w=[('wb', n, j)], dma='cast_' + n)

    later_casts = []
    if "B" in phases:
        later_casts += [(lambda n=n, j=j: cast_rows_j(n, j)) for n in ("w_in", "w_out") for j in range(4)]
    if "C" in phases:
        later_casts += [(lambda fg=fg: cast_ffn_fg("ffn2", fg)) for fg in range(KF // 4)]
    if "A" not in phases:
        for f_ in later_casts:
            f_()
        later_casts = []

    def wkeys(n):
        return [('wb', n, j) for j in range(4)]

    pbank = [0]

    def nb():
        b = pbank[0]
        pbank[0] = (b + 1) % 6
        return b

    def ffn_phase(tag, src, dst, n_g, n_u, n_d, g_pre_d, g_post_d):
        A.reset()
        ntiles = ntok // 512
        big = A.t("big", [128, 4, D], F32)
        xres = [A.t("xres", [128, D], F32) for _ in range(2)]
        xnT = A.t("xnT", [128, KD, 512], BF16)
        actT = [A.t("actT", [128, 4, 512], BF16) for _ in range(2)]
        wg = [A.t("wg", [128, KD, 512], BF16) for _ in range(2)]
        wu = [A.t("wu", [128, KD, 512], BF16) for _ in range(2)]
        wd = [A.t("wd", [128, 4, D], BF16) for _ in range(2)]
        gpre = A.t("gpre", [128, D], F32)
        gpost = A.t("gpost", [128, D], F32)
        xn_tm = [A.t("xn_tm", [128, D], BF16) for _ in range(2)]
        sgt = [A.t("sgt", [128, 512], F32) for _ in range(2)]
        ss = A.t("ss", [128, 8], F32)
        rstd = A.t("rstd", [128, 8], F32)
        K = lambda *a: (tag,) + a
        wgb, wub, wdb = wb[n_g], wb[n_u], wb[n_d]

        S.add('sp', lambda e: e.dma_start(out=gpre[:], in_=g_pre_d.partition_broadcast(128)), w=[K('gpre')], dma=tag + 'g')
        S.add('sp', lambda e: e.dma_start(out=gpost[:], in_=g_post_d.partition_broadcast(128)), w=[K('gpost')], dma=tag + 'g')

        NG = KF // 4
        steps = [(t, fg) for t in range(ntiles) for fg in range(NG)]
        xr_cnt = [0]

        def load_gu(i):
            t, fg = steps[i]
            sl = i % 2
            S.add('sp', lambda e: e.dma_start(out=wg[sl][:], in_=wgb[:, fg * 512:(fg + 1) * 512].rearrange("(k p) c -> p k c", p=128)),
                  r=[('wb', n_g, fg)], w=[K('wg', sl), ('ctok', n_g, fg % 4)], dma=tag + 'wg%d' % sl)
            S.add('sp', lambda e: e.dma_start(out=wu[sl][:], in_=wub[:, fg * 512:(fg + 1) * 512].rearrange("(k p) c -> p k c", p=128)),
                  r=[('wb', n_u, fg)], w=[K('wu', sl), ('ctok', n_u, fg % 4)], dma=tag + 'wu%d' % sl)

        def load_d(i):
            t, fg = steps[i]
            sl = i % 2
            S.add('sp', lambda e: e.dma_start(out=wd[sl][:], in_=wdb[fg * 512:(fg + 1) * 512, :].rearrange("(c p) d -> p c d", p=128)),
                  r=[('wb', n_d, fg)], w=[K('wd', sl), ('ctok', n_d, fg % 4)], dma=tag + 'wd%d' % sl)

        def norm_stats(srcs, col0, n):
            S.add('pool', lambda e: e.memset(ss[:, col0:col0 + n], 0.0), w=[K('ss', col0)])
            for j, (ap, keys, junk, jkeys) in enumerate(srcs):
                S.add('act', lambda e, ap=ap, junk=junk, j=j: e.activation(out=junk, in_=ap, func=AF.Square,
                                                                            accum_out=ss[:, col0 + j:col0 + j + 1]),
                      r=list(keys) + [K('ss', col0)], w=list(jkeys) + [K('ss', col0)])

            S.add('act', lambda e: e.activation(out=rstd[:, col0:col0 + n], in_=ss[:, col0:col0 + n], func=AF.Sqrt,
                                                bias=epsc[:, 0:1], scale=1.0 / D),
                  r=[K('ss', col0), 'epsc'], w=[K('rstd', col0)])
            S.add('dve', lambda e: e.reciprocal(out=rstd[:, col0:col0 + n], in_=rstd[:, col0:col0 + n]),
                  r=[K('rstd', col0)], w=[K('rstd', col0)])

        def prologue(t):
            for s in range(4):
                sl = xr_cnt[0] % 2
                xr_cnt[0] += 1
                r0 = t * 512 + s * 128
                S.add('sp', lambda e, sl=sl, r0=r0: e.dma_start(out=xres[sl][:], in_=src[r0:r0 + 128, :]),
                      r=[('dram', tag + 'src', r0)], w=[K('xres', sl)], dma=tag + 'xr%d' % sl)
                m = s % 2
                norm_stats([(xres[sl][:], [K('xres', sl)], xn_tm[m][:], [K('xn_tm', m)])], s, 1)
                S.add('dve', lambda e, sl=sl, m=m, s=s: e.scalar_tensor_tensor(
                    out=xn_tm[m][:], in0=xres[sl][:], scalar=rstd[:, s:s + 1], in1=gpre[:], op0=ALU.mult, op1=ALU.mult),
                    r=[K('xres', sl), K('rstd', s), K('gpre')], w=[K('xn_tm', m)])
                for hf in range(2):
                    tb = (2 * s + hf) % 2

                    def tr(e, m=m, hf=hf, tb=tb):
                        for j in range(8):
                            k = hf * 8 + j
                            ins = e.transpose(pst[tb][:, j * 128:(j + 1) * 128], xn_tm[m][:, k * 128:(k + 1) * 128], identb[:])
                        return ins
                    S.add('pe', tr, r=[K('xn_tm', m), 'identb'], w=[('pst', tb)])
                    S.add('dve', lambda e, hf=hf, tb=tb, s=s: e.tensor_copy(
                        out=xnT[:, hf * 8:(hf + 1) * 8, s * 128:(s + 1) * 128],
                        in_=pst[tb][:].rearrange("p (k c) -> p k c", c=128)),
                        r=[('pst', tb)], w=[K('xnT', s)])

        def gu(i):
            t, fg = steps[i]
            sl = i % 2
            for c in range(4):
                bg, bu = nb(), nb()

                def mm(e, c=c, bg=bg, bu=bu):
                    for k in range(KD):
                        e.matmul(psf[bg][:], lhsT=wg[sl][:, k, c * 128:(c + 1) * 128], rhs=xnT[:, k, :], start=(k == 0), stop=(k == KD - 1))
                    for k in range(KD):
                        ins = e.matmul(psf[bu][:], lhsT=wu[sl][:, k, c * 128:(c + 1) * 128], rhs=xnT[:, k, :], start=(k == 0), stop=(k == KD - 1))
                    return ins
                S.add('pe', mm, r=[K('wg', sl), K('wu', sl)] + [K('xnT', s) for s in range(4)], w=[('psf', bg), ('psf', bu)])
                p = c % 2
                S.add('act', lambda e, bg=bg, p=p: e.activation(out=sgt[p][:], in_=psf[bg][:], func=AF.Silu),
                      r=[('psf', bg)], w=[K('sgt', p)])
                S.add('dve', lambda e, bu=bu, p=p, c=c: e.tensor_tensor(out=actT[sl][:, c, :], in0=psf[bu][:], in1=sgt[p][:], op=ALU.mult),
                      r=[('psf', bu), K('sgt', p)], w=[K('actT', sl, c)])

        import os as _os2
        _dd = _os2.environ.get("DBG_DOWN", "")

        def down(i):
            t, fg = steps[i]
            sl = i % 2
            for dg in range(4):
                for s in range(4):
                    b = nb()

                    def mm(e, b=b, dg=dg, s=s):
                        for c in range(4):
                            ins = e.matmul(psf[b][:], lhsT=actT[sl][:, c, s * 128:(s + 1) * 128],
                                           rhs=wd[sl][:, c, dg * 512:(dg + 1) * 512], start=(c == 0), stop=(c == 3))
                        return ins
                    if _dd == "dma":
                        continue
                    S.add('pe', mm, r=[K('wd', sl)] + [K('actT', sl, c) for c in range(4)], w=[('psf', b)])
                    if _dd == "mm":
                        continue
                    dstap = big[:, s, dg * 512:(dg + 1) * 512]
                    if fg == 0:
                        S.add('dve', lambda e, b=b, dstap=dstap: e.tensor_copy(out=dstap, in_=psf[b][:]),
                              r=[('psf', b)], w=[K('big', s, dg)])
                    else:
                        S.add('dve', lambda e, b=b, dstap=dstap: e.tensor_tensor(out=dstap, in0=psf[b][:], in1=dstap, op=ALU.add),
                              r=[('psf', b), K('big', s, dg)], w=[K('big', s, dg)])

        def epilogue(t):
            for s in range(4):
                sl = xr_cnt[0] % 2
                xr_cnt[0] += 1
                r0 = t * 512 + s * 128
                S.add('sp', lambda e, sl=sl, r0=r0: e.dma_start(out=xres[sl][:], in_=src[r0:r0 + 128, :]),
                      r=[('dram', tag + 'src', r0)], w=[K('xres', sl)], dma=tag + 'xr%d' % sl)
                m = s % 2
                bkeys = [K('big', s, dg) for dg in range(4)]
                norm_stats([(big[:, s, :], bkeys, xn_tm[m][:], [K('xn_tm', m)])], 4 + s, 1)
                S.add('dve', lambda e, s=s: e.scalar_tensor_tensor(
                    out=big[:, s, :], in0=big[:, s, :], scalar=rstd[:, 4 + s:5 + s], in1=gpost[:], op0=ALU.mult, op1=ALU.mult),
                    r=bkeys + [K('rstd', 4 + s), K('gpost')], w=bkeys)
                S.add('dve', lambda e, s=s, sl=sl: e.scalar_tensor_tensor(
                    out=xres[sl][:], in0=big[:, s, :], scalar=0.5, in1=xres[sl][:], op0=ALU.mult, op1=ALU.add),
                    r=bkeys + [K('xres', sl)], w=[K('xres', sl)])
                S.add('sp', lambda e, sl=sl, r0=r0: e.dma_start(out=dst[r0:r0 + 128, :], in_=xres[sl][:]),
                      r=[K('xres', sl)], w=[('dram', tag + 'dst', r0)], dma=tag + 'xr%d' % sl)

        N = len(steps)
        own_cast = (tag == "A")
        if own_cast:
            cast_ffn_fg("ffn1", 0, True)
            cast_ffn_fg("ffn1", 1, True)
            cast_ffn_fg("ffn1", 2, True)
        import os as _os
        _stop = _os.environ.get("DBG_STOP", "")
        if _stop == "pro":
            prologue(0)
            return
        if _stop == "gu":
            load_gu(0)
            prologue(0)
            gu(0)
            return
        if _stop == "gud":
            load_gu(0)
            load_d(0)
            prologue(0)
            gu(0)
            down(0)
            return
        load_gu(0)
        load_d(0)
        if N > 1:
            load_gu(1)
        prologue(0)
        gu(0)
        for i in range(N):
            if own_cast:
                if i + 3 < NG:
                    cast_ffn_fg("ffn1", i + 3, True)
                elif later_casts:
                    later_casts.pop(0)()
            if i + 1 < N:
                load_d(i + 1)
            if i + 2 < N:
                load_gu(i + 2)
            if i + 1 < N:
                if steps[i + 1][1] == 0:
                    prologue(steps[i + 1][0])
                gu(i + 1)
            down(i)
            if steps[i][1] == NG - 1:
                epilogue(steps[i][0])
        if own_cast:
            while later_casts:
                later_casts.pop(0)()

    def mixer_phase(src, dst):
        A.reset()
        tag = "B"
        K = lambda *a: (tag,) + a
        NTB = ntok // 256
        winb, woutb = wb["w_in"], wb["w_out"]
        cB = A.t("cB", [128, 1664], F32)
        c2cur = A.t("c2cur", [128, 384], F32)
        S.add('sp', lambda e: e.dma_start(out=cB[:], in_=cst[:, 128:128 + 1664]), w=['cB'], dma='Bc')
        Tf, Uf, T4 = cB[:, 0:128], cB[:, 128:256], cB[:, 256:768]
        distc = [cB[:, 768:1024], cB[:, 1024:1280], cB[:, 1280:1536]]
        biasT = lambda h, m: cB[:, 1536 + h * 16 + m:1536 + h * 16 + m + 1]
        c2 = lambda qb, j: c2cur[:, j * 128:(j + 1) * 128]
        ens = A.t("ens", [8, 8, 128], BF16)
        onesb = A.t("onesb", [128, 128], BF16)
        S.add('pool', lambda e: e.memset(onesb[:], 1.0), w=['onesb'])
        gpre = A.t("gpre", [128, D], F32)
        gpost = A.t("gpost", [128, D], F32)
        bdecb = A.t("bdecb", [128, 512], F32)
        gnob = A.t("gnob", [128, 256], F32)
        wdus = A.t("wdus", [16, 512], F32)
        S.add('sp', lambda e: e.dma_start(out=gpre[:], in_=gains["mix_pre_norm"].partition_broadcast(128)), w=[K('gpre')], dma='Bg')
        S.add('sp', lambda e: e.dma_start(out=gpost[:], in_=gains["mix_post_norm"].partition_broadcast(128)), w=[K('gpost')], dma='Bg')
        S.add('sp', lambda e: e.dma_start(out=bdecb[:], in_=bdec.partition_broadcast(128)), w=['bdecb'], dma='Bg')
        S.add('sp', lambda e: e.dma_start(out=gnob[:], in_=gno.partition_broadcast(128)), w=['gnob'], dma='Bg')
        S.add('sp', lambda e: e.dma_start(out=wdus[:], in_=wdu), w=['wdus'], dma='Bg')
        hb = A.t("hb", [128, 2, D], F32)
        xn_tm = [A.t("xn_tm", [128, D], BF16) for _ in range(2)]
        xnT = A.t("xnT", [128, KD, 256], BF16)
        wst = [A.t("wst", [128, KD, 512], BF16) for _ in range(2)]
        glrT = A.t("glrT", [16, 256], F32)
        lae = A.t("lae", [128, D], F32)
        la = lae[:, 0:1024].rearrange("p (s c) -> p s c", c=512)
        eR = lae[:, 1024:2048].rearrange("p (s c) -> p s c", c=512)
        junk = A.t("junk", [128, D], BF16)
        e1T = A.t("e1T", [128, 4, 256], F32)
        e2T = A.t("e2T", [128, 4, 256], F32)
        qtT = A.t("qtT", [128, 4, 256], BF16)
        ktT = A.t("ktT", [128, 4, 256], BF16)
        khat = A.t("khat", [128, 2, 512], BF16)
        vt = A.t("vt", [128, 2, 1024], BF16)
        sg = A.t("sg", [128, 2, 1024], F32)
        Sst = A.t("Sst", [128, 4, 256], F32)
        Sb = A.t("Sb", [128, 4, 256], BF16)
        QT = A.t("QT", [128, 8, 256], BF16)
        KTt = A.t("KTt", [128, 8, 256], BF16)
        Vt = A.t("Vt", [128, 2, 1024], BF16)
        kmT = A.t("kmT", [128, 8, 8], F32)
        kmTb = A.t("kmTb", [128, 8, 8], BF16)
        KTp = [A.t("KTp", [128, 1792], BF16) for _ in range(2)]
        Vp = [A.t("Vp", [128, 14, 128], BF16) for _ in range(2)]
        gm = A.t("gm", [128, 128], F32)
        mx8 = A.t("mx8", [128, 16, 8], F32)
        sel = A.t("sel", [128, 128], F32)
        mbb = A.t("mbb", [128, 128], BF16)
        MBT = A.t("MBT", [8, 8, 256], BF16)
        ptile = [A.t("ptile", [128, 256], BF16) for _ in range(3)]
        ltmp = [A.t("ltmp", [128, 256], F32) for _ in range(2)]
        rec = [A.t("rec", [128, 256], F32) for _ in range(2)]
        mixT = A.t("mixT", [128, 16, 256], BF16)
        AT = [A.t("AT", [128, 4, 128], BF16) for _ in range(2)]
        ogb = A.t("ogb", [128, 1024], BF16)
        mst = A.t("mst", [128, D], F32)
        ogt = mst[:, 0:1024]
        ensf = mst[0:8, 0:1024]
        S.add('sp', lambda e: e.dma_start(out=ensf, in_=ensd), w=[K('mst', 0), K('mst', 1)], dma='Bc')
        S.add('dve', lambda e: e.tensor_copy(out=ens[:].rearrange("p n c -> p (n c)"), in_=ensf), r=[K('mst', 0), K('mst', 1)], w=['ens'])
        ss = A.t("ss", [128, 16], F32)
        rstd = A.t("rstd", [128, 16], F32)
        S.add('pool', lambda e: e.memset(kmT[:], 0.0), w=['kmT'])
        slopes = [2.0 ** (-(h + 1)) for h in range(8)]
        wcnt = [0]

        def rstd_of(col, n, scale):
            S.add('act', lambda e: e.activation(out=rstd[:, col:col + n], in_=ss[:, col:col + n], func=AF.Sqrt,
                                                bias=epsc[:, 0:1], scale=scale), r=[K('ss', col), 'epsc'], w=[K('rstd', col)])
            S.add('dve', lambda e: e.reciprocal(out=rstd[:, col:col + n], in_=rstd[:, col:col + n]),
                  r=[K('rstd', col)], w=[K('rstd', col)])

        def wload(srcb, c0, ncol):
            sl = wcnt[0] % 2
            wcnt[0] += 1
            S.add('sp', lambda e: e.dma_start(out=wst[sl][:, :, 0:ncol], in_=srcb[:, c0:c0 + ncol].rearrange("(k p) c -> p k c", p=128)),
                  r=wkeys("w_in") + wkeys("w_out"), w=[K('wst', sl)], dma='Bw%d' % sl)
            return sl

        def fm_mm(sl, c0, M, evac, bank=None):
            b = nb() if bank is None else bank

            def mm(e):
                for k in range(KD):
                    ins = e.matmul(psf[b][0:M, 0:256], lhsT=wst[sl][:, k, c0:c0 + M], rhs=xnT[:, k, :], start=(k == 0), stop=(k == KD - 1))
                return ins
            S.add('pe', mm, r=[K('wst', sl), K('xnT', 0), K('xnT', 1)], w=[('psf', b)])
            evac(b)

        def tm_mm(sl, s, evac):
            b = nb()

            def mm(e):
                for k in range(KD):
                    ins = e.matmul(psf[b][:], lhsT=xnT[:, k, s * 128:(s + 1) * 128], rhs=wst[sl][:, k, :], start=(k == 0), stop=(k == KD - 1))
                return ins
            S.add('pe', mm, r=[K('wst', sl), K('xnT', s)], w=[('psf', b)])
            evac(b)

        LAEK = [K('la', 0), K('la', 1), K('eR', 0), K('eR', 1)]
        preloaded = {}

        def pro1(tbn, s):
            r0 = tbn * 256 + s * 128
            S.add('sp', lambda e: e.dma_start(out=lae[:], in_=src[r0:r0 + 128, :]), r=[('dram', 'Bsrc', tbn)], w=LAEK, dma='Bx')
            S.add('pool', lambda e: e.memset(ss[:, s:s + 1], 0.0), w=[K('ss', s)])
            S.add('act', lambda e: e.activation(out=junk[:], in_=lae[:], func=AF.Square, accum_out=ss[:, s:s + 1]),
                  r=LAEK + [K('ss', s)], w=[K('junk'), K('ss', s)])
            rstd_of(s, 1, 1.0 / D)
            S.add('dve', lambda e: e.scalar_tensor_tensor(out=xn_tm[s][:], in0=lae[:], scalar=rstd[:, s:s + 1], in1=gpre[:],
                                                          op0=ALU.mult, op1=ALU.mult),
                  r=LAEK + [K('rstd', s), K('gpre')], w=[K('xn_tm', s)])

        def pro2(s):
            for hf in range(2):
                def tr(e, hf=hf):
                    for j in range(8):
                        k = hf * 8 + j
                        ins = e.transpose(pst[hf][:, j * 128:(j + 1) * 128], xn_tm[s][:, k * 128:(k + 1) * 128], identb[:])
                    return ins
                S.add('pe', tr, r=[K('xn_tm', s), 'identb'], w=[('pst', hf)])
                S.add('dve', lambda e, hf=hf: e.tensor_copy(out=xnT[:, hf * 8:(hf + 1) * 8, s * 128:(s + 1) * 128],
                                                            in_=pst[hf][:].rearrange("p (k c) -> p k c", c=128)),
                      r=[('pst', hf)], w=[K('xnT', s)])

        def do_tile(tb):
            tok0 = tb * 256
            qb = tb % 8
            seq0 = (tb // 8) * SEQ
            if qb == 0:
                S.add('pool', lambda e: e.memset(Sst[:], 0.0), w=[K('Sst', h) for h in range(4)])
                S.add('pool', lambda e: e.memset(Sb[:], 0.0), w=[K('Sb')])
            S.add('sp', lambda e: e.dma_start(out=c2cur[:], in_=cst[:, 128 + 1664 + qb * 384:128 + 1664 + (qb + 1) * 384]), w=['c2cur'], dma='Bc2')
            if tb == 0:
                pro1(0, 0)
                pro2(0)
                pro1(0, 1)
                pro2(1)
            sl = preloaded.pop('lr') if 'lr' in preloaded else wload(winb, C_LR, 16)
            fm_mm(sl, 0, 16, lambda b: S.add('dve', lambda e: e.tensor_copy(out=glrT[:], in_=psf[b][0:16, 0:256]),
                                              r=[('psf', b)], w=['glrT']))
            for s in range(2):
                b = nb()
                S.add('pe', lambda e, b=b, s=s: e.matmul(psf[b][:], lhsT=glrT[:, s * 128:(s + 1) * 128], rhs=wdus[:], start=True, stop=True),
                      r=['glrT', 'wdus'], w=[('psf', b)])
                S.add('dve', lambda e, b=b, s=s: e.tensor_tensor(out=la[:, s, :], in0=psf[b][:], in1=bdecb[:], op=ALU.add),
                      r=[('psf', b), 'bdecb'], w=[K('la', s)])
                S.add('act', lambda e, s=s: e.activation(out=la[:, s, :], in_=la[:, s, :], func=AF.Exp, scale=-1.0), r=[K('la', s)], w=[K('la', s)])
                S.add('act', lambda e, s=s: e.activation(out=la[:, s, :], in_=la[:, s, :], func=AF.Ln, bias=onec[:, 0:1]), r=[K('la', s), 'onec'], w=[K('la', s)])
            for j in range(2):
                sl = preloaded.pop('gv0') if (j == 0 and 'gv0' in preloaded) else wload(winb, C_GV + j * 512, 512)
                for s in range(2):
                    tm_mm(sl, s, lambda b, s=s, j=j: S.add('dve', lambda e: e.tensor_copy(out=vt[:, s, j * 512:(j + 1) * 512], in_=psf[b][:]),
                                                           r=[('psf', b)], w=[K('vt', s, j)]))
            for j in range(2):
                sl = wload(winb, C_GG + j * 512, 512)
                for s in range(2):
                    tm_mm(sl, s, lambda b, s=s, j=j: S.add('act', lambda e: e.activation(out=sg[:, s, j * 512:(j + 1) * 512], in_=psf[b][:], func=AF.Silu),
                                                           r=[('psf', b)], w=[K('sg', s, j)]))
            for j in range(2):
                sl = wload(winb, C_MQ + j * 512, 512)
                for hh in range(4):
                    h = j * 4 + hh
                    fm_mm(sl, hh * 128, 128, lambda b, h=h: S.add('dve', lambda e: e.tensor_scalar(
                        out=QT[:, h, :], in0=psf[b][:, 0:256], scalar1=128.0 ** -0.5, scalar2=None, op0=ALU.mult),
                        r=[('psf', b)], w=[K('QT', h)]))
            for j in range(2):
                sl = wload(winb, C_MK + j * 512, 512)
                for hh in range(4):
                    h = j * 4 + hh

                    def ev(b, h=h):
                        S.add('dve', lambda e: e.tensor_copy(out=KTt[:, h, :], in_=psf[b][:, 0:256]), r=[('psf', b)], w=[K('KTt', h)])
                        S.add('dve', lambda e: e.tensor_reduce(out=kmT[:, h, qb:qb + 1], in_=psf[b][:, 0:256], axis=AX.X, op=ALU.add),
                              r=[('psf', b)], w=['kmT'])
                    fm_mm(sl, hh * 128, 128, ev)
            for j in range(2):
                sl = wload(winb, C_MV + j * 512, 512)
                for s in range(2):
                    tm_mm(sl, s, lambda b, s=s, j=j: S.add('dve', lambda e: e.tensor_copy(out=Vt[:, s, j * 512:(j + 1) * 512], in_=psf[b][:]),
                                                           r=[('psf', b)], w=[K('Vt', s, j)]))
            for s in range(2):
                b = nb()

                def cum(e, b=b, s=s):
                    for h in range(4):
                        ins = e.matmul(psf[b][:, h * 128:(h + 1) * 128], lhsT=la[:, s, h * 128:(h + 1) * 128], rhs=Tf, start=True, stop=True)
                    return ins
                S.add('pe', cum, r=[K('la', s), 'cB'], w=[('psf', b)])
                S.add('act', lambda e, b=b, s=s: e.activation(out=e1T[:, :, s * 128:(s + 1) * 128], in_=psf[b][:].rearrange("p (h c) -> p h c", c=128),
                                                              func=AF.Exp, scale=-1.0 / 16.0), r=[('psf', b)], w=[K('e1T', s)])
                S.add('act', lambda e, b=b, s=s: e.activation(out=e2T[:, :, s * 128:(s + 1) * 128], in_=psf[b][:].rearrange("p (h c) -> p h c", c=128),
                                                              func=AF.Exp, scale=1.0 / 16.0), r=[('psf', b)], w=[K('e2T', s)])
                b = nb()
                S.add('pe', lambda e, b=b, s=s: e.matmul(psf[b][:], lhsT=Uf, rhs=la[:, s, :], start=True, stop=True),
                      r=[K('la', s), 'cB'], w=[('psf', b)])
                S.add('act', lambda e, b=b, s=s: e.activation(out=eR[:, s, :], in_=psf[b][:], func=AF.Exp, scale=-1.0 / 16.0),
                      r=[('psf', b)], w=[K('eR', s)])
            E12 = [K('e1T', 0), K('e1T', 1)]
            E22 = [K('e2T', 0), K('e2T', 1)]
            sl = wload(winb, C_GQ, 512)
            for h in range(4):
                fm_mm(sl, h * 128, 128, lambda b, h=h: S.add('dve', lambda e: e.scalar_tensor_tensor(
                    out=qtT[:, h, :], in0=psf[b][:, 0:256], scalar=128.0 ** -0.5, in1=e1T[:, h, :], op0=ALU.mult, op1=ALU.mult),
                    r=[('psf', b)] + E12, w=[K('qtT', h)]))
            sl = wload(winb, C_GK, 512)
            for h in range(4):
                fm_mm(sl, h * 128, 128, lambda b, h=h: S.add('dve', lambda e: e.tensor_tensor(
                    out=ktT[:, h, :], in0=psf[b][:, 0:256], in1=e2T[:, h, :], op=ALU.mult), r=[('psf', b)] + E22, w=[K('ktT', h)]))
            for s in range(2):
                tm_mm(sl, s, lambda b, s=s: S.add('dve', lambda e: e.tensor_tensor(out=khat[:, s, :], in0=psf[b][:], in1=eR[:, s, :], op=ALU.mult),
                                                  r=[('psf', b), K('eR', s)], w=[K('khat', s)]))
            S.add('sp', lambda e: e.dma_start(out=hb[:], in_=src[tok0:tok0 + 256, :].rearrange("(s p) d -> p s d", p=128)),
                  r=[('dram', 'Bsrc', tb)], w=[K('hb', 0), K('hb', 1)], dma='Bh')
            KTtk = [K('KTt', h) for h in range(8)]
            Vtk = [K('Vt', s, j) for s in range(2) for j in range(2)]
            if qb < 7:
                S.add('pool', lambda e, tok0=tok0: e.dma_start(out=KTd[:, :, tok0:tok0 + 256].rearrange("h d t -> d h t"), in_=KTt[:]),
                      r=KTtk, w=[('dram', 'KTd', tb)], dma='Bkc')
                S.add('pool', lambda e, tok0=tok0: e.dma_start(out=Vd[tok0:tok0 + 256, :].rearrange("(s p) c -> p s c", p=128), in_=Vt[:]),
                      r=Vtk, w=[('dram', 'Vd', tb)], dma='Bkc')
            S.add('dve', lambda e: e.tensor_copy(out=kmTb[:], in_=kmT[:]), r=['kmT'], w=['kmTb'])

            bgt = nb()

            def gmm(e, bgt=bgt):
                for s in range(2):
                    for h in range(8):
                        g = s * 8 + h
                        ins = e.matmul(psf[bgt][:, g * 8:(g + 1) * 8], lhsT=QT[:, h, s * 128:(s + 1) * 128], rhs=kmTb[:, h, :], start=True, stop=True)
                return ins
            S.add('pe', gmm, r=[K('QT', h) for h in range(8)] + ['kmTb'], w=[('psf', bgt)])
            S.add('dve', lambda e, bgt=bgt, qb=qb: e.tensor_tensor(out=gm[:], in0=psf[bgt][:, 0:128], in1=c2(qb, 0), op=ALU.add),
                  r=[('psf', bgt), 'c2cur'], w=['gm'])

            def selop1(e):
                for g in range(16):
                    ins = e.max(out=mx8[:, g, :], in_=gm[:, g * 8:(g + 1) * 8])
                return ins
            S.add('dve', selop1, r=['gm'], w=['mx8'])

            def selop2(e):
                for g in range(16):
                    ins = e.tensor_scalar(out=sel[:, g * 8:(g + 1) * 8], in0=gm[:, g * 8:(g + 1) * 8], scalar1=mx8[:, g, 2:3], scalar2=None, op0=ALU.is_ge)
                return ins
            S.add('dve', selop2, r=['gm', 'mx8'], w=['sel'])
            S.add('dve', lambda e, qb=qb: e.tensor_tensor(out=sel[:], in0=sel[:], in1=c2(qb, 1), op=ALU.mult), r=['sel', 'c2cur'], w=['sel'])
            S.add('dve', lambda e, qb=qb: e.scalar_tensor_tensor(out=mbb[:], in0=sel[:], scalar=32768.0, in1=c2(qb, 2), op0=ALU.mult, op1=ALU.add),
                  r=['sel', 'c2cur'], w=['mbb'])

            def trm(e):
                for s in range(2):
                    for h in range(8):
                        g = s * 8 + h
                        ins = e.transpose(pst[h // 4][0:8, (h % 4) * 256 + s * 128:(h % 4) * 256 + (s + 1) * 128], mbb[:, g * 8:(g + 1) * 8], identb[:])
                return ins
            S.add('pe', trm, r=['mbb', 'identb'], w=[('pst', 0), ('pst', 1)])
            for q in range(2):
                S.add('dve', lambda e, q=q: e.tensor_copy(out=MBT[:, q * 4:(q + 1) * 4, :].rearrange("p h c -> p (h c)"), in_=pst[q][0:8, :]),
                      r=[('pst', q)], w=[K('MBT', q)])

            if tb + 1 < NTB:
                pro1(tb + 1, 0)
            CS = [slice(0, 128), slice(128, 256)]
            bsc, bds, bos = {}, {}, {}
            for s in range(2):
                cs = CS[s]
                b = nb()
                bsc[s] = b

                def sc(e, b=b, cs=cs):
                    for h in range(4):
                        ins = e.matmul(psf[b][:, h * 128:(h + 1) * 128], lhsT=ktT[:, h, cs], rhs=qtT[:, h, cs], start=True, stop=True)
                    return ins
                S.add('pe', sc, r=[K('ktT', h) for h in range(4)] + [K('qtT', h) for h in range(4)], w=[('psf', b)])
            for s in range(2):
                bd = [nb(), nb()]
                bds[s] = bd

                def dsm(e, bd=bd, s=s):
                    for h in range(4):
                        ins = e.matmul(psf[bd[h // 2]][:, (h % 2) * 256:(h % 2 + 1) * 256], lhsT=khat[:, s, h * 128:(h + 1) * 128],
                                       rhs=vt[:, s, h * 256:(h + 1) * 256], start=True, stop=True)
                    return ins
                S.add('pe', dsm, r=[K('khat', s), K('vt', s, 0), K('vt', s, 1)], w=[('psf', bd[0]), ('psf', bd[1])])
            for s in range(2):
                S.add('dve', lambda e, s=s: e.tensor_tensor(out=AT[s][:].rearrange("p h c -> p (h c)"), in0=psf[bsc[s]][:], in1=T4, op=ALU.mult),
                      r=[('psf', bsc[s]), 'cB'], w=[K('AT', s)])
            for s in range(2):
                cs = CS[s]
                bo = [nb(), nb()]
                bd = bds[s]

                def om(e, bo=bo, cs=cs, s=s):
                    for h in range(4):
                        o_ap = psf[bo[h // 2]][:, (h % 2) * 256:(h % 2 + 1) * 256]
                        e.matmul(o_ap, lhsT=AT[s][:, h, :], rhs=vt[:, s, h * 256:(h + 1) * 256], start=True, stop=False)
                        ins = e.matmul(o_ap, lhsT=qtT[:, h, cs], rhs=Sb[:, h, :], start=False, stop=True)
                    return ins
                S.add('pe', om, r=[K('AT', s), K('vt', s, 0), K('vt', s, 1), K('Sb')] + [K('qtT', h) for h in range(4)],
                      w=[('psf', bo[0]), ('psf', bo[1])])
                for h in range(4):
                    S.add('dve', lambda e, h=h, bd=bd, s=s: e.scalar_tensor_tensor(
                        out=Sst[:, h, :], in0=Sst[:, h, :], scalar=e1T[:, h, s * 128 + 127:s * 128 + 128],
                        in1=psf[bd[h // 2]][:, (h % 2) * 256:(h % 2 + 1) * 256], op0=ALU.mult, op1=ALU.add),
                        r=[K('Sst', h), K('e1T', s), ('psf', bd[h // 2])], w=[K('Sst', h)])
                S.add('pool', lambda e: e.tensor_copy(out=Sb[:], in_=Sst[:]), r=[K('Sst', h) for h in range(4)], w=[K('Sb')])
                S.add('pool', lambda e: e.memset(ss[:, 4:8], 0.0), w=[K('ss', 4)])
                for h in range(4):
                    S.add('act', lambda e, h=h, bo=bo: e.activation(out=junk[:, h * 256:(h + 1) * 256],
                                                                    in_=psf[bo[h // 2]][:, (h % 2) * 256:(h % 2 + 1) * 256],
                                                                    func=AF.Square, accum_out=ss[:, 4 + h:5 + h]),
                          r=[('psf', bo[h // 2]), K('ss', 4)], w=[K('junk'), K('ss', 4)])
                rstd_of(4, 4, 1.0 / 256.0)
                for h in range(4):
                    S.add('dve', lambda e, h=h, bo=bo: e.scalar_tensor_tensor(
                        out=mst[:, h * 256:(h + 1) * 256], in0=psf[bo[h // 2]][:, (h % 2) * 256:(h % 2 + 1) * 256],
                        scalar=rstd[:, 4 + h:5 + h], in1=gnob[:], op0=ALU.mult, op1=ALU.mult),
                        r=[('psf', bo[h // 2]), K('rstd', 4), 'gnob'], w=[K('mst', h // 2)])
                S.add('dve', lambda e, s=s: e.tensor_tensor(out=ogb[:], in0=mst[:, 0:1024], in1=sg[:, s, :], op=ALU.mult),
                      r=[K('mst', 0), K('mst', 1), K('sg', s, 0), K('sg', s, 1)], w=[K('ogb')])

                def trg(e):
                    for c in range(8):
                        ins = e.transpose(pst[0][:, c * 128:(c + 1) * 128], ogb[:, c * 128:(c + 1) * 128], identb[:])
                    return ins
                S.add('pe', trg, r=[K('ogb'), 'identb'], w=[('pst', 0)])
                S.add('dve', lambda e, cs=cs: e.tensor_copy(out=mixT[:, 0:8, cs], in_=pst[0][:].rearrange("p (k c) -> p k c", c=128)),
                      r=[('pst', 0)], w=[K('mixT', 'g', cs.start)])

            LAG = 2
            nkt = 2 * (qb + 1)
            items = [(h, kt) for h in range(8) for kt in range(nkt)]
            info = {}

            def emit_loads(h):
                hp = h % 2
                if qb > 0:
                    S.add('pool', lambda e: e.dma_start(out=KTp[hp][:, 0:qb * 256], in_=KTd[h, :, seq0:seq0 + qb * 256]),
                          r=[('dram', 'KTd', tb - 1 - i) for i in range(qb)], w=[K('KTp', hp)], dma='Bkp%d' % hp)
                    S.add('pool', lambda e: e.dma_start(out=Vp[hp][:, 0:2 * qb, :],
                                                        in_=Vd[seq0:seq0 + qb * 256, h * 128:(h + 1) * 128].rearrange("(k p) d -> p k d", p=128)),
                          r=[('dram', 'Vd', tb - 1 - i) for i in range(qb)], w=[K('Vp', hp)], dma='Bvp%d' % hp)

            def emit_s(idx):
                h, kt = items[idx]
                hp = h % 2
                n = kt // 2
                if n == qb:
                    ko = kt - 2 * qb
                    kT_ap = KTt[:, h, ko * 128:(ko + 1) * 128]
                    v_ap = Vt[:, ko, h * 128:(h + 1) * 128]
                    rk, rv = [K('KTt', h)], [K('Vt', ko, h // 4)]
                    dc, m = distc[1 + ko], 0
                else:
                    kT_ap = KTp[hp][:, kt * 128:(kt + 1) * 128]
                    v_ap = Vp[hp][:, kt, :]
                    rk, rv = [K('KTp', hp)], [K('Vp', hp)]
                    dc, m = distc[0], 2 * qb - kt
                bS, pt = idx % 2, idx % 3
                info[idx] = (v_ap, rv, pt)

                def smm(e):
                    e.matmul(psf[bS][:, 0:256], lhsT=kT_ap, rhs=QT[:, h, :], start=True, stop=False)
                    return e.matmul(psf[bS][:, 0:256], lhsT=ens[:, n, :], rhs=MBT[:, h, :], start=False, stop=True)
                S.add('pe', smm, r=rk + [K('QT', h), K('MBT', h // 4), 'ens'], w=[('psf', bS)])
                S.add('dve', lambda e: e.scalar_tensor_tensor(out=ltmp[bS][:], in0=dc, scalar=-slopes[h], in1=psf[bS][:, 0:256],
                                                              op0=ALU.mult, op1=ALU.add),
                      r=[('psf', bS), 'cB'], w=[K('ltmp', bS)])
                S.add('act', lambda e: e.activation(out=ptile[pt][:], in_=ltmp[bS][:], func=AF.Exp, bias=biasT(h, m)),
                      r=[K('ltmp', bS), 'cB'], w=[K('ptile', pt)])

            def emit_pv(idx):
                h, kt = items[idx]
                hp = h % 2
                bO, bL = 2 + 2 * hp, 3 + 2 * hp
                v_ap, rv, pt = info[idx]

                def pv(e):
                    e.matmul(psf[bO][:, 0:256], lhsT=v_ap, rhs=ptile[pt][:], start=(kt == 0), stop=(kt == nkt - 1))
                    return e.matmul(psf[bL][:, 0:256], lhsT=onesb[:], rhs=ptile[pt][:], start=(kt == 0), stop=(kt == nkt - 1))
                S.add('pe', pv, r=rv + [K('ptile', pt), 'onesb'], w=[('psf', bO), ('psf', bL)])
                if kt == nkt - 1:
                    S.add('dve', lambda e: e.reciprocal(out=rec[hp][:], in_=psf[bL][:, 0:256]), r=[('psf', bL)], w=[K('rec', hp)])
                    S.add('dve', lambda e: e.tensor_tensor(out=mixT[:, 8 + h, :], in0=psf[bO][:, 0:256], in1=rec[hp][:], op=ALU.mult),
                          r=[('psf', bO), K('rec', hp)], w=[K('mixT', 'm', h)])

            emit_loads(0)
            emit_loads(1)
            for idx in range(len(items)):
                h, kt = items[idx]
                if kt == 0 and h >= 1 and h + 1 < 8:
                    emit_loads(h + 1)
                if kt == 0 and tb + 1 < NTB:
                    if h == 2:
                        pro2(0)
                        pro1(tb + 1, 1)
                    if h == 5:
                        pro2(1)
                emit_s(idx)
                if idx >= LAG:
                    emit_pv(idx - LAG)
            for idx in range(max(0, len(items) - LAG), len(items)):
                emit_pv(idx)
            pbank[0] = 0
            mixk = [K('mixT', 'g', 0), K('mixT', 'g', 128)] + [K('mixT', 'm', h) for h in range(8)]
            MS = [mst, lae]
            MK = [[K('mst', j) for j in range(4)], LAEK]
            for j in range(4):
                sl = wload(woutb, j * 512, 512)
                for s in range(2):
                    b = nb()

                    def mmo(e, b=b, s=s, sl=sl):
                        for k in range(KD):
                            ins = e.matmul(psf[b][:], lhsT=mixT[:, k, s * 128:(s + 1) * 128], rhs=wst[sl][:, k, :], start=(k == 0), stop=(k == KD - 1))
                        return ins
                    S.add('pe', mmo, r=mixk + [K('wst', sl)], w=[('psf', b)])
                    S.add('dve', lambda e, b=b, j=j, s=s: e.tensor_copy(out=MS[s][:, j * 512:(j + 1) * 512], in_=psf[b][:]),
                          r=[('psf', b)], w=([K('mst', j)] if s == 0 else LAEK))
            if tb + 1 < NTB:
                preloaded['lr'] = wload(winb, C_LR, 16)
                preloaded['gv0'] = wload(winb, C_GV, 512)
            for s in range(2):
                mk = MK[s]
                S.add('pool', lambda e, s=s: e.memset(ss[:, 8 + s:9 + s], 0.0), w=[K('ss', 8 + s)])
                S.add('act', lambda e, s=s: e.activation(out=junk[:], in_=MS[s][:], func=AF.Square, accum_out=ss[:, 8 + s:9 + s]),
                      r=mk + [K('ss', 8 + s)], w=[K('junk'), K('ss', 8 + s)])
                rstd_of(8 + s, 1, 1.0 / D)
                S.add('dve', lambda e, s=s: e.scalar_tensor_tensor(out=MS[s][:], in0=MS[s][:], scalar=rstd[:, 8 + s:9 + s], in1=gpost[:],
                                                                   op0=ALU.mult, op1=ALU.mult), r=mk + [K('rstd', 8 + s), K('gpost')], w=mk)
                S.add('dve', lambda e, s=s: e.tensor_tensor(out=hb[:, s, :], in0=MS[s][:], in1=hb[:, s, :], op=ALU.add),
                      r=mk + [K('hb', s)], w=[K('hb', s)])
                S.add('sp', lambda e, s=s: e.dma_start(out=dst[tok0 + s * 128:tok0 + (s + 1) * 128, :], in_=hb[:, s, :]),
                      r=[K('hb', s)], w=[('dram', 'Bdst', tb, s)], dma='Bo')

        for tb_ in range(NTB):
            do_tile(tb_)

    if "A" in phases:
        ffn_phase("A", x, h1 if ("B" in phases or "C" in phases) else out, "ffn1_w_gate", "ffn1_w_up", "ffn1_w_down",
                  gains["ffn1_pre_norm"], gains["ffn1_post_norm"])
        S.barrier()
    if "B" in phases:
        mixer_phase(h1 if "A" in phases else x, h2 if "C" in phases else out)
        S.barrier()
    if "C" in phases:
        srcC = h2 if "B" in phases else (h1 if "A" in phases else x)
        ffn_phase("C", srcC, out, "ffn2_w_gate", "ffn2_w_up", "ffn2_w_down",
                  gains["ffn2_pre_norm"], gains["ffn2_post_norm"])
    ok, mx = S.simulate()
    assert ok, 'sync deadlock'
    S.emit()
    return nc


NCB = 1792 + 3072


def make_consts():
    c = np.zeros((128, 128 + NCB), np.float32)
    c[:, 0:128] = np.eye(128, dtype=np.float32)
    b = c[:, 128:]
    j = np.arange(128)[:, None]
    i = np.arange(128)[None, :]
    T = (j <= i).astype(np.float32)
    b[:, 0:128] = T
    b[:, 128:256] = (j > i).astype(np.float32)
    b[:, 256:768] = np.tile(T, (1, 4))
    t = np.arange(256)[None, :]
    sl = np.arange(128)[:, None]
    b[:, 768:1024] = (t - sl).astype(np.float32)
    for kt in range(2):
        dd = (t - (kt * 128 + sl)).astype(np.float32)
        b[:, 1024 + kt * 256:1280 + kt * 256] = np.where(dd >= 0, dd, 1e6)
    slopes = 2.0 ** (-8.0 * np.arange(1, 9) / 8.0)
    for h in range(8):
        for m in range(16):
            b[:, 1536 + h * 16 + m] = -slopes[h] * m * 128.0
    for qb in range(8):
        n = np.arange(8)
        pastneg = np.where(n < qb, 0.0, -1e30).astype(np.float32)
        past01 = (n < qb).astype(np.float32)
        c3 = ((n == qb).astype(np.float32) - 1.0) * 32768.0
        base = 1664 + qb * 384
        b[:, base:base + 128] = np.tile(pastneg, 16)[None, :]
        b[:, base + 128:base + 256] = np.tile(past01, 16)[None, :]
        b[:, base + 256:base + 384] = np.tile(c3, 16)[None, :]
    return c


def make_ens():
    e = np.zeros((8, 1024), np.float32)
    for n in range(8):
        e[n, n * 128:(n + 1) * 128] = 1.0
    return e


_NC_CACHE = {}


def kernel(**inputs):
    x = np.ascontiguousarray(inputs["x"], dtype=np.float32)
    B = x.shape[0]
    per = B // NCORES
    ntok = per * SEQ
    key = ntok
    if key not in _NC_CACHE:
        _NC_CACHE[key] = build_program(ntok)
    nc = _NC_CACHE[key]
    shared = {}
    for n in ("ffn1_w_gate", "ffn1_w_up", "ffn1_w_down", "ffn2_w_gate", "ffn2_w_up", "ffn2_w_down", "w_in", "w_out"):
        shared[n] = np.ascontiguousarray(inputs[n][0], dtype=np.float32)
    for n in ("ffn1_pre_norm", "ffn1_post_norm", "mix_pre_norm", "mix_post_norm", "ffn2_pre_norm", "ffn2_post_norm",
              "gla_b_decay", "gla_out_norm"):
        shared[n] = np.ascontiguousarray(inputs[n], dtype=np.float32).reshape(1, -1)
    shared["gla_w_decay_up"] = np.ascontiguousarray(inputs["gla_w_decay_up"][0], dtype=np.float32)
    shared["consts"] = make_consts()
    shared["ens"] = make_ens()
    in_maps = []
    for c in range(NCORES):
        m = dict(shared)
        m["x"] = x[c * per:(c + 1) * per].reshape(ntok, D)
        in_maps.append(m)
    res = run_bass_kernel_spmd(nc, in_maps, core_ids=list(range(NCORES)))
    outs = [np.asarray(r["out"]).reshape(per, SEQ, D) for r in res.results]
    return np.concatenate(outs, axis=0).astype(np.float32)
```

```python
import bisect
import contextlib
import numpy as np
import concourse.bass as bass
import concourse.mybir as mybir
from concourse.bass_utils import run_bass_kernel_spmd

F32 = mybir.dt.float32
BF16 = mybir.dt.bfloat16
AF = mybir.ActivationFunctionType
ALU = mybir.AluOpType
AX = mybir.AxisListType

D = 2048
F = 5632
KD = D // 128
KF = F // 128
SEQ = 2048
NSEQ = 2
INW = 6160
EPS = 1e-6
NCORES = 8

C_GQ, C_GK, C_GV, C_GG, C_LR, C_MQ, C_MK, C_MV = 0, 512, 1024, 2048, 3072, 3088, 4112, 5136


class Sch:
    def __init__(self, nc):
        self.nc = nc
        self.ops = []
        self.lastw = {}
        self.readers = {}

    def add(self, eng, fn, r=(), w=(), dma=None):
        i = len(self.ops)
        deps = set()
        for k in r:
            if k in self.lastw:
                deps.add(self.lastw[k])
        for k in w:
            if k in self.lastw:
                deps.add(self.lastw[k])
            for j in self.readers.get(k, ()):
                deps.add(j)
        self.ops.append(dict(eng=eng, fn=fn, deps=deps, dma=dma))
        for k in r:
            self.readers.setdefault(k, []).append(i)
        for k in w:
            self.lastw[k] = i
            self.readers[k] = []
        return i

    def barrier(self):
        n = len(self.ops)
        last = {}
        for i, o in enumerate(self.ops):
            if o['fn'] is not None and not o['dma']:
                last[o['eng']] = i
        alld = set(last.values()) | {i for i, o in enumerate(self.ops) if o['dma']}
        for e in ['sp', 'act', 'dve', 'pool', 'pe']:
            self.ops.append(dict(eng=e, fn=None, deps=set(alld), dma=None, bar=True))
        self.lastw = {}
        self.readers = {}

    def plan(self, final_wait_eng='sp'):
        ops = self.ops
        engs = ['sp', 'act', 'dve', 'pool', 'pe']
        dma_sems = sorted({o['dma'] for o in ops if o['dma']})
        dma_idx = {s: [i for i, o in enumerate(ops) if o['dma'] == s] for s in dma_sems}
        milestone = [False] * len(ops)
        for i, o in enumerate(ops):
            for d in o['deps']:
                od = ops[d]
                if od['dma']:
                    continue
                if od['eng'] == o['eng'] and o['eng'] == 'pe' and not o['dma'] and not o.get('bar'):
                    continue
                if od['eng'] == o['eng'] and o.get('bar'):
                    continue
                milestone[d] = True
        ms_index = {}
        cnt = {e: 0 for e in engs}
        for i, o in enumerate(ops):
            if o['dma'] or o['fn'] is None:
                continue
            if milestone[i]:
                cnt[o['eng']] += 1
                ms_index[i] = cnt[o['eng']]
        plan = {e: [] for e in engs}
        waited = {e: {} for e in engs}
        for i, o in enumerate(ops):
            engname = o['eng']
            need = {}
            for d in o['deps']:
                od = ops[d]
                if od['dma']:
                    s = od['dma']
                    v = 16 * bisect.bisect_left(dma_idx[s], i)
                    key = ('d', s)
                else:
                    if od['eng'] == engname and engname == 'pe' and not o['dma']:
                        continue
                    if od['eng'] == engname and o.get('bar'):
                        continue
                    key = ('e', od['eng'])
                    v = ms_index[d]
                if v > need.get(key, 0):
                    need[key] = v
            waits = []
            for key, v in need.items():
                if waited[engname].get(key, 0) >= v:
                    continue
                waited[engname][key] = v
                waits.append((key, v))
            inc = None
            if o['fn'] is not None:
                if o['dma']:
                    inc = (('d', o['dma']), 16)
                elif milestone[i]:
                    inc = (('e', engname), 1)
            plan[engname].append((waits, i, inc))
        fin = [(('d', s), 16 * len(dma_idx[s])) for s in dma_sems]
        fin += [(('e', en), cnt[en]) for en in engs if cnt[en] > 0 and en != final_wait_eng]
        plan[final_wait_eng].append((fin, None, None))
        return plan, dma_sems

    def simulate(self):
        plan, _ = self.plan()
        sem = {}
        pos = {e: 0 for e in plan}
        progress = True
        while progress:
            progress = False
            for e, lst in plan.items():
                while pos[e] < len(lst):
                    waits, i, inc = lst[pos[e]]
                    if all(sem.get(k, 0) >= v for k, v in waits):
                        if inc:
                            sem[inc[0]] = sem.get(inc[0], 0) + inc[1]
                        pos[e] += 1
                        progress = True
                    else:
                        break
        stuck = {e: (pos[e], len(lst)) for e, lst in plan.items() if pos[e] < len(lst)}
        if stuck:
            for e in stuck:
                waits, i, inc = plan[e][pos[e]]
                print("STUCK", e, pos[e], len(plan[e]), [(k, v, sem.get(k, 0)) for k, v in waits])
        return not stuck, max(sem.values()) if sem else 0

    def emit(self, final_wait_eng='sp'):
        nc = self.nc
        ops = self.ops
        engs = ['sp', 'act', 'dve', 'pool', 'pe']
        plan, dma_sems = self.plan(final_wait_eng)
        with contextlib.ExitStack() as st:
            esem = {e: st.enter_context(nc.semaphore('s_' + e)) for e in engs}
            dsem = {s: st.enter_context(nc.semaphore('d_' + s)) for s in dma_sems}
            getsem = lambda key: dsem[key[1]] if key[0] == 'd' else esem[key[1]]
            block = st.enter_context(nc.Block())

            def make(engname):
                def body(e):
                    for waits, i, inc in plan[engname]:
                        for key, v in waits:
                            e.wait_ge(getsem(key), v)
                        if i is None or ops[i]['fn'] is None:
                            continue
                        ins = ops[i]['fn'](e)
                        if inc:
                            ins.then_inc(getsem(inc[0]), inc[1])
                return body

            block.sync(make('sp'))
            block.scalar(make('act'))
            block.vector(make('dve'))
            block.gpsimd(make('pool'))
            block.tensor(make('pe'))


class Arena:
    BASE = 16512
    LIMIT = 229344

    def __init__(self, nc):
        self.nc = nc
        self.cur = self.BASE
        self.n = 0
        self.mark_ = self.BASE

    def t(self, name, shape, dtype):
        esz = 4 if dtype == F32 else 2
        nbytes = esz
        for s in shape[1:]:
            nbytes *= s
        self.cur = (self.cur + 63) // 64 * 64
        off = self.cur
        self.cur += nbytes
        assert self.cur <= self.LIMIT, (name, self.cur - self.BASE)
        self.n += 1
        return self.nc.alloc_sbuf_tensor_at("%s_%d" % (name, self.n), list(shape), dtype, offset=off)

    def mark(self):
        self.mark_ = self.cur

    def reset(self):
        self.cur = self.mark_


def build_program(ntok=4096, phases=("A", "B", "C"), dbg=False):
    nc = bass.Bass("TRN2", target_bir_lowering=False)
    dt = lambda n, s, d=F32, k="ExternalInput": nc.dram_tensor(n, list(s), d, kind=k).ap()
    x = dt("x", [ntok, D])
    w_f32 = {}
    for pfx in ("ffn1", "ffn2"):
        w_f32[pfx + "_w_gate"] = dt(pfx + "_w_gate", [D, F])
        w_f32[pfx + "_w_up"] = dt(pfx + "_w_up", [D, F])
        w_f32[pfx + "_w_down"] = dt(pfx + "_w_down", [F, D])
    w_f32["w_in"] = dt("w_in", [D, INW])
    w_f32["w_out"] = dt("w_out", [D, D])
    gains = {n: dt(n, [1, D]) for n in ("ffn1_pre_norm", "ffn1_post_norm", "mix_pre_norm", "mix_post_norm",
                                         "ffn2_pre_norm", "ffn2_post_norm")}
    wdu = dt("gla_w_decay_up", [16, 512])
    bdec = dt("gla_b_decay", [1, 512])
    gno = dt("gla_out_norm", [1, 256])
    cst = dt("consts", [128, 128 + NCB])
    ensd = dt("ens", [8, 1024])
    KTd = dt("KTd", [8, 128, ntok], BF16, "Internal")
    Vd = dt("Vd", [ntok, 1024], BF16, "Internal")
    out = dt("out", [ntok, D], F32, "ExternalOutput")
    wb = {n: dt(n + "_b", ap.shape, BF16, "Internal") for n, ap in w_f32.items()}
    h1 = dt("h1", [ntok, D], F32, "Internal")
    h2 = dt("h2", [ntok, D], F32, "Internal")

    S = Sch(nc)
    A = Arena(nc)
    psf = [nc.alloc_psum_tensor("psf%d" % i, [128, 512], F32) for i in range(6)]
    pst = [nc.alloc_psum_tensor("pst%d" % i, [128, 1024], BF16) for i in range(2)]

    cf = A.t("cf", [128, 128], F32)
    identb = A.t("identb", [128, 128], BF16)
    S.add('sp', lambda e: e.dma_start(out=cf[:], in_=cst[:, 0:128]), w=['cf'], dma='cst')
    S.add('dve', lambda e: e.tensor_copy(out=identb[:], in_=cf[:, 0:128]), r=['cf'], w=['identb'])
    epsc = A.t("epsc", [128, 1], F32)
    S.add('pool', lambda e: e.memset(epsc[:], EPS), w=['epsc'])
    onec = A.t("onec", [128, 1], F32)
    S.add('pool', lambda e: e.memset(onec[:], 1.0), w=['onec'])
    A.mark()

    def cast_ffn_fg(pfx, fg, rot=False):
        g, u, d = pfx + "_w_gate", pfx + "_w_up", pfx + "_w_down"
        c0, c1 = fg * 512, (fg + 1) * 512
        sfx = ("_%d" % (fg % 4)) if rot else ""
        tok = (lambda n: [('ctok', n, fg % 4)]) if rot else (lambda n: [])
        S.add('pool', lambda e: e.dma_start(out=wb[g][:, c0:c1], in_=w_f32[g][:, c0:c1]), w=[('wb', g, fg)] + tok(g), dma='cast_' + g + sfx)
        S.add('pool', lambda e: e.dma_start(out=wb[u][:, c0:c1], in_=w_f32[u][:, c0:c1]), w=[('wb', u, fg)] + tok(u), dma='cast_' + u + sfx)
        S.add('pool', lambda e: e.dma_start(out=wb[d][c0:c1, :], in_=w_f32[d][c0:c1, :]), w=[('wb', d, fg)] + tok(d), dma='cast_' + d + sfx)

    def cast_rows_j(n, j):
        src, dst = w_f32[n], wb[n]
        step = src.shape[0] // 4
        a, b = j * step, (j + 1) * step
        S.add('pool', lambda e: e.dma_start(out=dst[a:b, :], in_=src[a:b, :]), w=[('wb', n, j)], dma='cast_' + n)

    later_casts = []
    if "B" in phases:
        later_casts += [(lambda n=n, j=j: cast_rows_j(n, j)) for n in ("w_in", "w_out") for j in range(4)]
    if "C" in phases:
        later_casts += [(lambda fg=fg: cast_ffn_fg("ffn2", fg)) for fg in range(KF // 4)]
    if "A" not in phases:
        for f_ in later_casts:
            f_()
        later_casts = []

    def wkeys(n):
        return [('wb', n, j) for j in range(4)]

    pbank = [0]

    def nb():
        b = pbank[0]
        pbank[0] = (b + 1) % 6
        return b

    def ffn_phase(tag, src, dst, n_g, n_u, n_d, g_pre_d, g_post_d):
        A.reset()
        ntiles = ntok // 512
        big = A.t("big", [128, 4, D], F32)
        xres = [A.t("xres", [128, D], F32) for _ in range(2)]
        xnT = A.t("xnT", [128, KD, 512], BF16)
        actT = [A.t("actT", [128, 4, 512], BF16) for _ in range(2)]
        wg = [A.t("wg", [128, KD, 512], BF16) for _ in range(2)]
        wu = [A.t("wu", [128, KD, 512], BF16) for _ in range(2)]
        wd = [A.t("wd", [128, 4, D], BF16) for _ in range(2)]
        gpre = A.t("gpre", [128, D], F32)
        gpost = A.t("gpost", [128, D], F32)
        xn_tm = [A.t("xn_tm", [128, D], BF16) for _ in range(2)]
        sgt = [A.t("sgt", [128, 512], F32) for _ in range(2)]
        ss = A.t("ss", [128, 8], F32)
        rstd = A.t("rstd", [128, 8], F32)
        K = lambda *a: (tag,) + a
        wgb, wub, wdb = wb[n_g], wb[n_u], wb[n_d]

        S.add('sp', lambda e: e.dma_start(out=gpre[:], in_=g_pre_d.partition_broadcast(128)), w=[K('gpre')], dma=tag + 'g')
        S.add('sp', lambda e: e.dma_start(out=gpost[:], in_=g_post_d.partition_broadcast(128)), w=[K('gpost')], dma=tag + 'g')

        NG = KF // 4
        steps = [(t, fg) for t in range(ntiles) for fg in range(NG)]
        xr_cnt = [0]

        def load_gu(i):
            t, fg = steps[i]
            sl = i % 2
            S.add('sp', lambda e: e.dma_start(out=wg[sl][:], in_=wgb[:, fg * 512:(fg + 1) * 512].rearrange("(k p) c -> p k c", p=128)),
                  r=[('wb', n_g, fg)], w=[K('wg', sl), ('ctok', n_g, fg % 4)], dma=tag + 'wg%d' % sl)
            S.add('sp', lambda e: e.dma_start(out=wu[sl][:], in_=wub[:, fg * 512:(fg + 1) * 512].rearrange("(k p) c -> p k c", p=128)),
                  r=[('wb', n_u, fg)], w=[K('wu', sl), ('ctok', n_u, fg % 4)], dma=tag + 'wu%d' % sl)

        def load_d(i):
            t, fg = steps[i]
            sl = i % 2
            S.add('sp', lambda e: e.dma_start(out=wd[sl][:], in_=wdb[fg * 512:(fg + 1) * 512, :].rearrange("(c p) d -> p c d", p=128)),
                  r=[('wb', n_d, fg)], w=[K('wd', sl), ('ctok', n_d, fg % 4)], dma=tag + 'wd%d' % sl)

        def norm_stats(srcs, col0, n):
            S.add('pool', lambda e: e.memset(ss[:, col0:col0 + n], 0.0), w=[K('ss', col0)])
            for j, (ap, keys, junk, jkeys) in enumerate(srcs):
                S.add('act', lambda e, ap=ap, junk=junk, j=j: e.activation(out=junk, in_=ap, func=AF.Square,
                                                                            accum_out=ss[:, col0 + j:col0 + j + 1]),
                      r=list(keys) + [K('ss', col0)], w=list(jkeys) + [K('ss', col0)])

            S.add('act', lambda e: e.activation(out=rstd[:, col0:col0 + n], in_=ss[:, col0:col0 + n], func=AF.Sqrt,
                                                bias=epsc[:, 0:1], scale=1.0 / D),
                  r=[K('ss', col0), 'epsc'], w=[K('rstd', col0)])
            S.add('dve', lambda e: e.reciprocal(out=rstd[:, col0:col0 + n], in_=rstd[:, col0:col0 + n]),
                  r=[K('rstd', col0)], w=[K('rstd', col0)])

        def prologue(t):
            for s in range(4):
                sl = xr_cnt[0] % 2
                xr_cnt[0] += 1
                r0 = t * 512 + s * 128
                S.add('sp', lambda e, sl=sl, r0=r0: e.dma_start(out=xres[sl][:], in_=src[r0:r0 + 128, :]),
                      r=[('dram', tag + 'src', r0)], w=[K('xres', sl)], dma=tag + 'xr%d' % sl)
                m = s % 2
                norm_stats([(xres[sl][:], [K('xres', sl)], xn_tm[m][:], [K('xn_tm', m)])], s, 1)
                S.add('dve', lambda e, sl=sl, m=m, s=s: e.scalar_tensor_tensor(
                    out=xn_tm[m][:], in0=xres[sl][:], scalar=rstd[:, s:s + 1], in1=gpre[:], op0=ALU.mult, op1=ALU.mult),
                    r=[K('xres', sl), K('rstd', s), K('gpre')], w=[K('xn_tm', m)])
                for hf in range(2):
                    tb = (2 * s + hf) % 2

                    def tr(e, m=m, hf=hf, tb=tb):
                        for j in range(8):
                            k = hf * 8 + j
                            ins = e.transpose(pst[tb][:, j * 128:(j + 1) * 128], xn_tm[m][:, k * 128:(k + 1) * 128], identb[:])
                        return ins
                    S.add('pe', tr, r=[K('xn_tm', m), 'identb'], w=[('pst', tb)])
                    S.add('dve', lambda e, hf=hf, tb=tb, s=s: e.tensor_copy(
                        out=xnT[:, hf * 8:(hf + 1) * 8, s * 128:(s + 1) * 128],
                        in_=pst[tb][:].rearrange("p (k c) -> p k c", c=128)),
                        r=[('pst', tb)], w=[K('xnT', s)])

        def gu(i):
            t, fg = steps[i]
            sl = i % 2
            for c in range(4):
                bg, bu = nb(), nb()

                def mm(e, c=c, bg=bg, bu=bu):
                    for k in range(KD):
                        e.matmul(psf[bg][:], lhsT=wg[sl][:, k, c * 128:(c + 1) * 128], rhs=xnT[:, k, :], start=(k == 0), stop=(k == KD - 1))
                    for k in range(KD):
                        ins = e.matmul(psf[bu][:], lhsT=wu[sl][:, k, c * 128:(c + 1) * 128], rhs=xnT[:, k, :], start=(k == 0), stop=(k == KD - 1))
                    return ins
                S.add('pe', mm, r=[K('wg', sl), K('wu', sl)] + [K('xnT', s) for s in range(4)], w=[('psf', bg), ('psf', bu)])
                p = c % 2
                S.add('act', lambda e, bg=bg, p=p: e.activation(out=sgt[p][:], in_=psf[bg][:], func=AF.Silu),
                      r=[('psf', bg)], w=[K('sgt', p)])
                S.add('dve', lambda e, bu=bu, p=p, c=c: e.tensor_tensor(out=actT[sl][:, c, :], in0=psf[bu][:], in1=sgt[p][:], op=ALU.mult),
                      r=[('psf', bu), K('sgt', p)], w=[K('actT', sl, c)])

        import os as _os2
        _dd = _os2.environ.get("DBG_DOWN", "")

        def down(i):
            t, fg = steps[i]
            sl = i % 2
            for dg in range(4):
                for s in range(4):
                    b = nb()

                    def mm(e, b=b, dg=dg, s=s):
                        for c in range(4):
                            ins = e.matmul(psf[b][:], lhsT=actT[sl][:, c, s * 128:(s + 1) * 128],
                                           rhs=wd[sl][:, c, dg * 512:(dg + 1) * 512], start=(c == 0), stop=(c == 3))
                        return ins
                    if _dd == "dma":
                        continue
                    S.add('pe', mm, r=[K('wd', sl)] + [K('actT', sl, c) for c in range(4)], w=[('psf', b)])
                    if _dd == "mm":
                        continue
                    dstap = big[:, s, dg * 512:(dg + 1) * 512]
                    if fg == 0:
                        S.add('dve', lambda e, b=b, dstap=dstap: e.tensor_copy(out=dstap, in_=psf[b][:]),
                              r=[('psf', b)], w=[K('big', s, dg)])
                    else:
                        S.add('dve', lambda e, b=b, dstap=dstap: e.tensor_tensor(out=dstap, in0=psf[b][:], in1=dstap, op=ALU.add),
                              r=[('psf', b), K('big', s, dg)], w=[K('big', s, dg)])

        def epilogue(t):
            for s in range(4):
                sl = xr_cnt[0] % 2
                xr_cnt[0] += 1
                r0 = t * 512 + s * 128
                S.add('sp', lambda e, sl=sl, r0=r0: e.dma_start(out=xres[sl][:], in_=src[r0:r0 + 128, :]),
                      r=[('dram', tag + 'src', r0)], w=[K('xres', sl)], dma=tag + 'xr%d' % sl)
                m = s % 2
                bkeys = [K('big', s, dg) for dg in range(4)]
                norm_stats([(big[:, s, :], bkeys, xn_tm[m][:], [K('xn_tm', m)])], 4 + s, 1)
                S.add('dve', lambda e, s=s: e.scalar_tensor_tensor(
                    out=big[:, s, :], in0=big[:, s, :], scalar=rstd[:, 4 + s:5 + s], in1=gpost[:], op0=ALU.mult, op1=ALU.mult),
                    r=bkeys + [K('rstd', 4 + s), K('gpost')], w=bkeys)
                S.add('dve', lambda e, s=s, sl=sl: e.scalar_tensor_tensor(
                    out=xres[sl][:], in0=big[:, s, :], scalar=0.5, in1=xres[sl][:], op0=ALU.mult, op1=ALU.add),
                    r=bkeys + [K('xres', sl)], w=[K('xres', sl)])
                S.add('sp', lambda e, sl=sl, r0=r0: e.dma_start(out=dst[r0:r0 + 128, :], in_=xres[sl][:]),
                      r=[K('xres', sl)], w=[('dram', tag + 'dst', r0)], dma=tag + 'xr%d' % sl)

        N = len(steps)
        own_cast = (tag == "A")
        if own_cast:
            cast_ffn_fg("ffn1", 0, True)
            cast_ffn_fg("ffn1", 1, True)
            cast_ffn_fg("ffn1", 2, True)
        import os as _os
        _stop = _os.environ.get("DBG_STOP", "")
        if _stop == "pro":
            prologue(0)
            return
        if _stop == "gu":
            load_gu(0)
            prologue(0)
            gu(0)
            return
        if _stop == "gud":
            load_gu(0)
            load_d(0)
            prologue(0)
            gu(0)
            down(0)
            return
        load_gu(0)
        load_d(0)
        if N > 1:
            load_gu(1)
        prologue(0)
        gu(0)
        for i in range(N):
            if own_cast:
                if i + 3 < NG:
                    cast_ffn_fg("ffn1", i + 3, True)
                elif later_casts:
                    later_casts.pop(0)()
            if i + 1 < N:
                load_d(i + 1)
            if i + 2 < N:
                load_gu(i + 2)
            if i + 1 < N:
                if steps[i + 1][1] == 0:
                    prologue(steps[i + 1][0])
                gu(i + 1)
            down(i)
            if steps[i][1] == NG - 1:
                epilogue(steps[i][0])
        if own_cast:
            while later_casts:
                later_casts.pop(0)()

    def mixer_phase(src, dst):
        A.reset()
        tag = "B"
        K = lambda *a: (tag,) + a
        NTB = ntok // 256
        winb, woutb = wb["w_in"], wb["w_out"]
        cB = A.t("cB", [128, 1664], F32)
        c2cur = A.t("c2cur", [128, 384], F32)
        S.add('sp', lambda e: e.dma_start(out=cB[:], in_=cst[:, 128:128 + 1664]), w=['cB'], dma='Bc')
        Tf, Uf, T4 = cB[:, 0:128], cB[:, 128:256], cB[:, 256:768]
        distc = [cB[:, 768:1024], cB[:, 1024:1280], cB[:, 1280:1536]]
        biasT = lambda h, m: cB[:, 1536 + h * 16 + m:1536 + h * 16 + m + 1]
        c2 = lambda qb, j: c2cur[:, j * 128:(j + 1) * 128]
        ens = A.t("ens", [8, 8, 128], BF16)
        onesb = A.t("onesb", [128, 128], BF16)
        S.add('pool', lambda e: e.memset(onesb[:], 1.0), w=['onesb'])
        gpre = A.t("gpre", [128, D], F32)
        gpost = A.t("gpost", [128, D], F32)
        bdecb = A.t("bdecb", [128, 512], F32)
        gnob = A.t("gnob", [128, 256], F32)
        wdus = A.t("wdus", [16, 512], F32)
        S.add('sp', lambda e: e.dma_start(out=gpre[:], in_=gains["mix_pre_norm"].partition_broadcast(128)), w=[K('gpre')], dma='Bg')
        S.add('sp', lambda e: e.dma_start(out=gpost[:], in_=gains["mix_post_norm"].partition_broadcast(128)), w=[K('gpost')], dma='Bg')
        S.add('sp', lambda e: e.dma_start(out=bdecb[:], in_=bdec.partition_broadcast(128)), w=['bdecb'], dma='Bg')
        S.add('sp', lambda e: e.dma_start(out=gnob[:], in_=gno.partition_broadcast(128)), w=['gnob'], dma='Bg')
        S.add('sp', lambda e: e.dma_start(out=wdus[:], in_=wdu), w=['wdus'], dma='Bg')
        hb = A.t("hb", [128, 2, D], F32)
        xn_tm = [A.t("xn_tm", [128, D], BF16) for _ in range(2)]
        xnT = A.t("xnT", [128, KD, 256], BF16)
        wst = [A.t("wst", [128, KD, 512], BF16) for _ in range(2)]
        glrT = A.t("glrT", [16, 256], F32)
        lae = A.t("lae", [128, D], F32)
        la = lae[:, 0:1024].rearrange("p (s c) -> p s c", c=512)
        eR = lae[:, 1024:2048].rearrange("p (s c) -> p s c", c=512)
        junk = A.t("junk", [128, D], BF16)
        e1T = A.t("e1T", [128, 4, 256], F32)
        e2T = A.t("e2T", [128, 4, 256], F32)
        qtT = A.t("qtT", [128, 4, 256], BF16)
        ktT = A.t("ktT", [128, 4, 256], BF16)
        khat = A.t("khat", [128, 2, 512], BF16)
        vt = A.t("vt", [128, 2, 1024], BF16)
        sg = A.t("sg", [128, 2, 1024], F32)
        Sst = A.t("Sst", [128, 4, 256], F32)
        Sb = A.t("Sb", [128, 4, 256], BF16)
        QT = A.t("QT", [128, 8, 256], BF16)
        KTt = A.t("KTt", [128, 8, 256], BF16)
        Vt = A.t("Vt", [128, 2, 1024], BF16)
        kmT = A.t("kmT", [128, 8, 8], F32)
        kmTb = A.t("kmTb", [128, 8, 8], BF16)
        KTp = [A.t("KTp", [128, 1792], BF16) for _ in range(2)]
        Vp = [A.t("Vp", [128, 14, 128], BF16) for _ in range(2)]
        gm = A.t("gm", [128, 128], F32)
        mx8 = A.t("mx8", [128, 16, 8], F32)
        sel = A.t("sel", [128, 128], F32)
        mbb = A.t("mbb", [128, 128], BF16)
        MBT = A.t("MBT", [8, 8, 256], BF16)
        ptile = [A.t("ptile", [128, 256], BF16) for _ in range(3)]
        ltmp = [A.t("ltmp", [128, 256], F32) for _ in range(2)]
        rec = [A.t("rec", [128, 256], F32) for _ in range(2)]
        mixT = A.t("mixT", [128, 16, 256], BF16)
        AT = [A.t("AT", [128, 4, 128], BF16) for _ in range(2)]
        ogb = [A.t("ogb", [128, 1024], BF16) for _ in range(2)]
        mst = A.t("mst", [128, D], F32)
        ogt = mst[:, 0:1024]
        ensf = mst[0:8, 0:1024]
        S.add('sp', lambda e: e.dma_start(out=ensf, in_=ensd), w=[K('mst', 0), K('mst', 1)], dma='Bc')
        S.add('dve', lambda e: e.tensor_copy(out=ens[:].rearrange("p n c -> p (n c)"), in_=ensf), r=[K('mst', 0), K('mst', 1)], w=['ens'])
        ss = A.t("ss", [128, 16], F32)
        rstd = A.t("rstd", [128, 16], F32)
        S.add('pool', lambda e: e.memset(kmT[:], 0.0), w=['kmT'])
        slopes = [2.0 ** (-(h + 1)) for h in range(8)]
        wcnt = [0]

        def rstd_of(col, n, scale):
            S.add('act', lambda e: e.activation(out=rstd[:, col:col + n], in_=ss[:, col:col + n], func=AF.Sqrt,
                                                bias=epsc[:, 0:1], scale=scale), r=[K('ss', col), 'epsc'], w=[K('rstd', col)])
            S.add('dve', lambda e: e.reciprocal(out=rstd[:, col:col + n], in_=rstd[:, col:col + n]),
                  r=[K('rstd', col)], w=[K('rstd', col)])

        def wload(srcb, c0, ncol):
            sl = wcnt[0] % 2
            wcnt[0] += 1
            S.add('sp', lambda e: e.dma_start(out=wst[sl][:, :, 0:ncol], in_=srcb[:, c0:c0 + ncol].rearrange("(k p) c -> p k c", p=128)),
                  r=wkeys("w_in") + wkeys("w_out"), w=[K('wst', sl)], dma='Bw%d' % sl)
            return sl

        def fm_mm(sl, c0, M, evac, bank=None):
            b = nb() if bank is None else bank

            def mm(e):
                for k in range(KD):
                    ins = e.matmul(psf[b][0:M, 0:256], lhsT=wst[sl][:, k, c0:c0 + M], rhs=xnT[:, k, :], start=(k == 0), stop=(k == KD - 1))
                return ins
            S.add('pe', mm, r=[K('wst', sl), K('xnT', 0), K('xnT', 1)], w=[('psf', b)])
            evac(b)

        def tm_mm(sl, s, evac):
            b = nb()

            def mm(e):
                for k in range(KD):
                    ins = e.matmul(psf[b][:], lhsT=xnT[:, k, s * 128:(s + 1) * 128], rhs=wst[sl][:, k, :], start=(k == 0), stop=(k == KD - 1))
                return ins
            S.add('pe', mm, r=[K('wst', sl), K('xnT', s)], w=[('psf', b)])
            evac(b)

        LAEK = [K('la', 0), K('la', 1), K('eR', 0), K('eR', 1)]
        preloaded = {}

        def pro1(tbn, s):
            r0 = tbn * 256 + s * 128
            S.add('sp', lambda e: e.dma_start(out=lae[:], in_=src[r0:r0 + 128, :]), r=[('dram', 'Bsrc', tbn)], w=LAEK, dma='Bx')
            S.add('pool', lambda e: e.memset(ss[:, s:s + 1], 0.0), w=[K('ss', s)])
            S.add('act', lambda e: e.activation(out=junk[:], in_=lae[:], func=AF.Square, accum_out=ss[:, s:s + 1]),
                  r=LAEK + [K('ss', s)], w=[K('junk'), K('ss', s)])
            rstd_of(s, 1, 1.0 / D)
            S.add('dve', lambda e: e.scalar_tensor_tensor(out=xn_tm[s][:], in0=lae[:], scalar=rstd[:, s:s + 1], in1=gpre[:],
                                                          op0=ALU.mult, op1=ALU.mult),
                  r=LAEK + [K('rstd', s), K('gpre')], w=[K('xn_tm', s)])

        def pro2(s):
            for hf in range(2):
                def tr(e, hf=hf):
                    for j in range(8):
                        k = hf * 8 + j
                        ins = e.transpose(pst[hf][:, j * 128:(j + 1) * 128], xn_tm[s][:, k * 128:(k + 1) * 128], identb[:])
                    return ins
                S.add('pe', tr, r=[K('xn_tm', s), 'identb'], w=[('pst', hf)])
                S.add('dve', lambda e, hf=hf: e.tensor_copy(out=xnT[:, hf * 8:(hf + 1) * 8, s * 128:(s + 1) * 128],
                                                            in_=pst[hf][:].rearrange("p (k c) -> p k c", c=128)),
                      r=[('pst', hf)], w=[K('xnT', s)])

        def do_tile(tb):
            tok0 = tb * 256
            qb = tb % 8
            seq0 = (tb // 8) * SEQ
            if qb == 0:
                S.add('pool', lambda e: e.memset(Sst[:], 0.0), w=[K('Sst', h) for h in range(4)])
                S.add('pool', lambda e: e.memset(Sb[:], 0.0), w=[K('Sb')])
            S.add('sp', lambda e: e.dma_start(out=c2cur[:], in_=cst[:, 128 + 1664 + qb * 384:128 + 1664 + (qb + 1) * 384]), w=['c2cur'], dma='Bc2')
            if tb == 0:
                pro1(0, 0)
                pro2(0)
                pro1(0, 1)
                pro2(1)
            sl = preloaded.pop('lr') if 'lr' in preloaded else wload(winb, C_LR, 16)
            fm_mm(sl, 0, 16, lambda b: S.add('dve', lambda e: e.tensor_copy(out=glrT[:], in_=psf[b][0:16, 0:256]),
                                              r=[('psf', b)], w=['glrT']))
            for s in range(2):
                b = nb()
                S.add('pe', lambda e, b=b, s=s: e.matmul(psf[b][:], lhsT=glrT[:, s * 128:(s + 1) * 128], rhs=wdus[:], start=True, stop=True),
                      r=['glrT', 'wdus'], w=[('psf', b)])
                S.add('dve', lambda e, b=b, s=s: e.tensor_tensor(out=la[:, s, :], in0=psf[b][:], in1=bdecb[:], op=ALU.add),
                      r=[('psf', b), 'bdecb'], w=[K('la', s)])
                S.add('act', lambda e, s=s: e.activation(out=la[:, s, :], in_=la[:, s, :], func=AF.Exp, scale=-1.0), r=[K('la', s)], w=[K('la', s)])
                S.add('act', lambda e, s=s: e.activation(out=la[:, s, :], in_=la[:, s, :], func=AF.Ln, bias=onec[:, 0:1]), r=[K('la', s), 'onec'], w=[K('la', s)])
            for j in range(2):
                sl = preloaded.pop('gv0') if (j == 0 and 'gv0' in preloaded) else wload(winb, C_GV + j * 512, 512)
                for s in range(2):
                    tm_mm(sl, s, lambda b, s=s, j=j: S.add('dve', lambda e: e.tensor_copy(out=vt[:, s, j * 512:(j + 1) * 512], in_=psf[b][:]),
                                                           r=[('psf', b)], w=[K('vt', s, j)]))
            for j in range(2):
                sl = wload(winb, C_GG + j * 512, 512)
                for s in range(2):
                    tm_mm(sl, s, lambda b, s=s, j=j: S.add('act', lambda e: e.activation(out=sg[:, s, j * 512:(j + 1) * 512], in_=psf[b][:], func=AF.Silu),
                                                           r=[('psf', b)], w=[K('sg', s, j)]))
            for j in range(2):
                sl = wload(winb, C_MQ + j * 512, 512)
                for hh in range(4):
                    h = j * 4 + hh
                    fm_mm(sl, hh * 128, 128, lambda b, h=h: S.add('dve', lambda e: e.tensor_scalar(
                        out=QT[:, h, :], in0=psf[b][:, 0:256], scalar1=128.0 ** -0.5, scalar2=None, op0=ALU.mult),
                        r=[('psf', b)], w=[K('QT', h)]))
            for j in range(2):
                sl = wload(winb, C_MK + j * 512, 512)
                for hh in range(4):
                    h = j * 4 + hh

                    def ev(b, h=h):
                        S.add('dve', lambda e: e.tensor_copy(out=KTt[:, h, :], in_=psf[b][:, 0:256]), r=[('psf', b)], w=[K('KTt', h)])
                        S.add('dve', lambda e: e.tensor_reduce(out=kmT[:, h, qb:qb + 1], in_=psf[b][:, 0:256], axis=AX.X, op=ALU.add),
                              r=[('psf', b)], w=['kmT'])
                    fm_mm(sl, hh * 128, 128, ev)
            for j in range(2):
                sl = wload(winb, C_MV + j * 512, 512)
                for s in range(2):
                    tm_mm(sl, s, lambda b, s=s, j=j: S.add('dve', lambda e: e.tensor_copy(out=Vt[:, s, j * 512:(j + 1) * 512], in_=psf[b][:]),
                                                           r=[('psf', b)], w=[K('Vt', s, j)]))
            for s in range(2):
                b = nb()

                def cum(e, b=b, s=s):
                    for h in range(4):
                        ins = e.matmul(psf[b][:, h * 128:(h + 1) * 128], lhsT=la[:, s, h * 128:(h + 1) * 128], rhs=Tf, start=True, stop=True)
                    return ins
                S.add('pe', cum, r=[K('la', s), 'cB'], w=[('psf', b)])
                S.add('act', lambda e, b=b, s=s: e.activation(out=e1T[:, :, s * 128:(s + 1) * 128], in_=psf[b][:].rearrange("p (h c) -> p h c", c=128),
                                                              func=AF.Exp, scale=-1.0 / 16.0), r=[('psf', b)], w=[K('e1T', s)])
                S.add('act', lambda e, b=b, s=s: e.activation(out=e2T[:, :, s * 128:(s + 1) * 128], in_=psf[b][:].rearrange("p (h c) -> p h c", c=128),
                                                              func=AF.Exp, scale=1.0 / 16.0), r=[('psf', b)], w=[K('e2T', s)])
                b = nb()
                S.add('pe', lambda e, b=b, s=s: e.matmul(psf[b][:], lhsT=Uf, rhs=la[:, s, :], start=True, stop=True),
                      r=[K('la', s), 'cB'], w=[('psf', b)])
                S.add('act', lambda e, b=b, s=s: e.activation(out=eR[:, s, :], in_=psf[b][:], func=AF.Exp, scale=-1.0 / 16.0),
                      r=[('psf', b)], w=[K('eR', s)])
            E12 = [K('e1T', 0), K('e1T', 1)]
            E22 = [K('e2T', 0), K('e2T', 1)]
            sl = wload(winb, C_GQ, 512)
            for h in range(4):
                fm_mm(sl, h * 128, 128, lambda b, h=h: S.add('dve', lambda e: e.scalar_tensor_tensor(
                    out=qtT[:, h, :], in0=psf[b][:, 0:256], scalar=128.0 ** -0.5, in1=e1T[:, h, :], op0=ALU.mult, op1=ALU.mult),
                    r=[('psf', b)] + E12, w=[K('qtT', h)]))
            sl = wload(winb, C_GK, 512)
            for h in range(4):
                fm_mm(sl, h * 128, 128, lambda b, h=h: S.add('dve', lambda e: e.tensor_tensor(
                    out=ktT[:, h, :], in0=psf[b][:, 0:256], in1=e2T[:, h, :], op=ALU.mult), r=[('psf', b)] + E22, w=[K('ktT', h)]))
            for s in range(2):
                tm_mm(sl, s, lambda b, s=s: S.add('dve', lambda e: e.tensor_tensor(out=khat[:, s, :], in0=psf[b][:], in1=eR[:, s, :], op=ALU.mult),
                                                  r=[('psf', b), K('eR', s)], w=[K('khat', s)]))
            S.add('sp', lambda e: e.dma_start(out=hb[:], in_=src[tok0:tok0 + 256, :].rearrange("(s p) d -> p s d", p=128)),
                  r=[('dram', 'Bsrc', tb)], w=[K('hb', 0), K('hb', 1)], dma='Bh')
            KTtk = [K('KTt', h) for h in range(8)]
            Vtk = [K('Vt', s, j) for s in range(2) for j in range(2)]
            if qb < 7:
                S.add('pool', lambda e, tok0=tok0: e.dma_start(out=KTd[:, :, tok0:tok0 + 256].rearrange("h d t -> d h t"), in_=KTt[:]),
                      r=KTtk, w=[('dram', 'KTd', tb)], dma='Bkc')
                S.add('pool', lambda e, tok0=tok0: e.dma_start(out=Vd[tok0:tok0 + 256, :].rearrange("(s p) c -> p s c", p=128), in_=Vt[:]),
                      r=Vtk, w=[('dram', 'Vd', tb)], dma='Bkc')
            S.add('dve', lambda e: e.tensor_copy(out=kmTb[:], in_=kmT[:]), r=['kmT'], w=['kmTb'])

            need_sel = qb > 3
            if need_sel:
                bgt = nb()

                def gmm(e, bgt=bgt):
                    for s in range(2):
                        for h in range(8):
                            g = s * 8 + h
                            ins = e.matmul(psf[bgt][:, g * 8:(g + 1) * 8], lhsT=QT[:, h, s * 128:(s + 1) * 128], rhs=kmTb[:, h, :], start=True, stop=True)
                    return ins
                S.add('pe', gmm, r=[K('QT', h) for h in range(8)] + ['kmTb'], w=[('psf', bgt)])
                S.add('dve', lambda e, bgt=bgt, qb=qb: e.tensor_tensor(out=gm[:], in0=psf[bgt][:, 0:128], in1=c2(qb, 0), op=ALU.add),
                      r=[('psf', bgt), 'c2cur'], w=['gm'])

                def selop1(e):
                    for g in range(16):
                        ins = e.max(out=mx8[:, g, :], in_=gm[:, g * 8:(g + 1) * 8])
                    return ins
                S.add('dve', selop1, r=['gm'], w=['mx8'])

                def selop2(e):
                    for g in range(16):
                        ins = e.tensor_scalar(out=sel[:, g * 8:(g + 1) * 8], in0=gm[:, g * 8:(g + 1) * 8], scalar1=mx8[:, g, 2:3], scalar2=None, op0=ALU.is_ge)
                    return ins
                S.add('dve', selop2, r=['gm', 'mx8'], w=['sel'])
                S.add('dve', lambda e, qb=qb: e.tensor_tensor(out=sel[:], in0=sel[:], in1=c2(qb, 1), op=ALU.mult), r=['sel', 'c2cur'], w=['sel'])
                S.add('dve', lambda e, qb=qb: e.scalar_tensor_tensor(out=mbb[:], in0=sel[:], scalar=32768.0, in1=c2(qb, 2), op0=ALU.mult, op1=ALU.add),
                      r=['sel', 'c2cur'], w=['mbb'])

            def sel_part2():
                def trm(e):
                    for s in range(2):
                        for h in range(8):
                            g = s * 8 + h
                            ins = e.transpose(pst[h // 4][0:8, (h % 4) * 256 + s * 128:(h % 4) * 256 + (s + 1) * 128], mbb[:, g * 8:(g + 1) * 8], identb[:])
                    return ins
                S.add('pe', trm, r=['mbb', 'identb'], w=[('pst', 0), ('pst', 1)])
                for q in range(2):
                    S.add('dve', lambda e, q=q: e.tensor_copy(out=MBT[:, q * 4:(q + 1) * 4, :].rearrange("p h c -> p (h c)"), in_=pst[q][0:8, :]),
                          r=[('pst', q)], w=[K('MBT', q)])

            if tb + 1 < NTB:
                pro1(tb + 1, 0)
            CS = [slice(0, 128), slice(128, 256)]
            bsc, bds, bos = {}, {}, {}
            for s in range(2):
                cs = CS[s]
                b = nb()
                bsc[s] = b

                def sc(e, b=b, cs=cs):
                    for h in range(4):
                        ins = e.matmul(psf[b][:, h * 128:(h + 1) * 128], lhsT=ktT[:, h, cs], rhs=qtT[:, h, cs], start=True, stop=True)
                    return ins
                S.add('pe', sc, r=[K('ktT', h) for h in range(4)] + [K('qtT', h) for h in range(4)], w=[('psf', b)])
            for s in range(2):
                bd = [nb(), nb()]
                bds[s] = bd

                def dsm(e, bd=bd, s=s):
                    for h in range(4):
                        ins = e.matmul(psf[bd[h // 2]][:, (h % 2) * 256:(h % 2 + 1) * 256], lhsT=khat[:, s, h * 128:(h + 1) * 128],
                                       rhs=vt[:, s, h * 256:(h + 1) * 256], start=True, stop=True)
                    return ins
                S.add('pe', dsm, r=[K('khat', s), K('vt', s, 0), K('vt', s, 1)], w=[('psf', bd[0]), ('psf', bd[1])])
            for s in range(2):
                S.add('dve', lambda e, s=s: e.tensor_tensor(out=AT[s][:].rearrange("p h c -> p (h c)"), in0=psf[bsc[s]][:], in1=T4, op=ALU.mult),
                      r=[('psf', bsc[s]), 'cB'], w=[K('AT', s)])
            for s in range(2):
                cs = CS[s]
                bo = [nb(), nb()]
                bd = bds[s]

                def om(e, bo=bo, cs=cs, s=s):
                    for h in range(4):
                        o_ap = psf[bo[h // 2]][:, (h % 2) * 256:(h % 2 + 1) * 256]
                        e.matmul(o_ap, lhsT=AT[s][:, h, :], rhs=vt[:, s, h * 256:(h + 1) * 256], start=True, stop=False)
                        ins = e.matmul(o_ap, lhsT=qtT[:, h, cs], rhs=Sb[:, h, :], start=False, stop=True)
                    return ins
                S.add('pe', om, r=[K('AT', s), K('vt', s, 0), K('vt', s, 1), K('Sb')] + [K('qtT', h) for h in range(4)],
                      w=[('psf', bo[0]), ('psf', bo[1])])
                for h in range(4):
                    S.add('dve', lambda e, h=h, bd=bd, s=s: e.scalar_tensor_tensor(
                        out=Sst[:, h, :], in0=Sst[:, h, :], scalar=e1T[:, h, s * 128 + 127:s * 128 + 128],
                        in1=psf[bd[h // 2]][:, (h % 2) * 256:(h % 2 + 1) * 256], op0=ALU.mult, op1=ALU.add),
                        r=[K('Sst', h), K('e1T', s), ('psf', bd[h // 2])], w=[K('Sst', h)])
                S.add('pool', lambda e: e.tensor_copy(out=Sb[:], in_=Sst[:]), r=[K('Sst', h) for h in range(4)], w=[K('Sb')])
                S.add('pool', lambda e: e.memset(ss[:, 4:8], 0.0), w=[K('ss', 4)])
                for h in range(4):
                    S.add('act', lambda e, h=h, bo=bo: e.activation(out=junk[:, h * 256:(h + 1) * 256],
                                                                    in_=psf[bo[h // 2]][:, (h % 2) * 256:(h % 2 + 1) * 256],
                                                                    func=AF.Square, accum_out=ss[:, 4 + h:5 + h]),
                          r=[('psf', bo[h // 2]), K('ss', 4)], w=[K('junk'), K('ss', 4)])
                rstd_of(4, 4, 1.0 / 256.0)
                for h in range(4):
                    S.add('dve', lambda e, h=h, bo=bo, s=s: e.scalar_tensor_tensor(
                        out=mst[:, s * 1024 + h * 256:s * 1024 + (h + 1) * 256], in0=psf[bo[h // 2]][:, (h % 2) * 256:(h % 2 + 1) * 256],
                        scalar=rstd[:, 4 + h:5 + h], in1=gnob[:], op0=ALU.mult, op1=ALU.mult),
                        r=[('psf', bo[h // 2]), K('rstd', 4), 'gnob'], w=[K('mst', 2 * s + h // 2)])
                S.add('dve', lambda e, s=s: e.tensor_tensor(out=ogb[s][:], in0=mst[:, s * 1024:(s + 1) * 1024], in1=sg[:, s, :], op=ALU.mult),
                      r=[K('mst', 2 * s), K('mst', 2 * s + 1), K('sg', s, 0), K('sg', s, 1)], w=[K('ogb', s)])

            def trg_emit(s):
                cs = CS[s]

                def trg(e):
                    for c in range(8):
                        ins = e.transpose(pst[0][:, c * 128:(c + 1) * 128], ogb[s][:, c * 128:(c + 1) * 128], identb[:])
                    return ins
                S.add('pe', trg, r=[K('ogb', s), 'identb'], w=[('pst', 0)])
                S.add('dve', lambda e: e.tensor_copy(out=mixT[:, 0:8, cs], in_=pst[0][:].rearrange("p (k c) -> p k c", c=128)),
                      r=[('pst', 0)], w=[K('mixT', 'g', cs.start)])

            if need_sel:
                sel_part2()
            LAG = 2
            nkt = 2 * (qb + 1)
            items = [(h, kt) for h in range(8) for kt in range(nkt)]
            info = {}

            def emit_loads(h):
                hp = h % 2
                if qb > 0:
                    S.add('pool', lambda e: e.dma_start(out=KTp[hp][:, 0:qb * 256], in_=KTd[h, :, seq0:seq0 + qb * 256]),
                          r=[('dram', 'KTd', tb - 1 - i) for i in range(qb)], w=[K('KTp', hp)], dma='Bkp%d' % hp)
                    S.add('pool', lambda e: e.dma_start(out=Vp[hp][:, 0:2 * qb, :],
                                                        in_=Vd[seq0:seq0 + qb * 256, h * 128:(h + 1) * 128].rearrange("(k p) d -> p k d", p=128)),
                          r=[('dram', 'Vd', tb - 1 - i) for i in range(qb)], w=[K('Vp', hp)], dma='Bvp%d' % hp)

            def emit_s(idx):
                h, kt = items[idx]
                hp = h % 2
                n = kt // 2
                if n == qb:
                    ko = kt - 2 * qb
                    kT_ap = KTt[:, h, ko * 128:(ko + 1) * 128]
                    v_ap = Vt[:, ko, h * 128:(h + 1) * 128]
                    rk, rv = [K('KTt', h)], [K('Vt', ko, h // 4)]
                    dc, m = distc[1 + ko], 0
                else:
                    kT_ap = KTp[hp][:, kt * 128:(kt + 1) * 128]
                    v_ap = Vp[hp][:, kt, :]
                    rk, rv = [K('KTp', hp)], [K('Vp', hp)]
                    dc, m = distc[0], 2 * qb - kt
                bS, pt = idx % 2, idx % 3
                info[idx] = (v_ap, rv, pt)

                masked = need_sel and n < qb

                def smm(e):
                    if not masked:
                        return e.matmul(psf[bS][:, 0:256], lhsT=kT_ap, rhs=QT[:, h, :], start=True, stop=True)
                    e.matmul(psf[bS][:, 0:256], lhsT=kT_ap, rhs=QT[:, h, :], start=True, stop=False)
                    return e.matmul(psf[bS][:, 0:256], lhsT=ens[:, n, :], rhs=MBT[:, h, :], start=False, stop=True)
                S.add('pe', smm, r=rk + [K('QT', h)] + ([K('MBT', h // 4), 'ens'] if masked else []), w=[('psf', bS)])
                S.add('dve', lambda e: e.scalar_tensor_tensor(out=ltmp[bS][:], in0=dc, scalar=-slopes[h], in1=psf[bS][:, 0:256],
                                                              op0=ALU.mult, op1=ALU.add),
                      r=[('psf', bS), 'cB'], w=[K('ltmp', bS)])
                S.add('act', lambda e: e.activation(out=ptile[pt][:], in_=ltmp[bS][:], func=AF.Exp, bias=biasT(h, m)),
                      r=[K('ltmp', bS), 'cB'], w=[K('ptile', pt)])

            def emit_pv(idx):
                h, kt = items[idx]
                hp = h % 2
                bO, bL = 2 + 2 * hp, 3 + 2 * hp
                v_ap, rv, pt = info[idx]

                def pv(e):
                    e.matmul(psf[bO][:, 0:256], lhsT=v_ap, rhs=ptile[pt][:], start=(kt == 0), stop=(kt == nkt - 1))
                    return e.matmul(psf[bL][:, 0:256], lhsT=onesb[:], rhs=ptile[pt][:], start=(kt == 0), stop=(kt == nkt - 1))
                S.add('pe', pv, r=rv + [K('ptile', pt), 'onesb'], w=[('psf', bO), ('psf', bL)])
                if kt == nkt - 1:
                    S.add('dve', lambda e: e.reciprocal(out=rec[hp][:], in_=psf[bL][:, 0:256]), r=[('psf', bL)], w=[K('rec', hp)])
                    S.add('dve', lambda e: e.tensor_tensor(out=mixT[:, 8 + h, :], in0=psf[bO][:, 0:256], in1=rec[hp][:], op=ALU.mult),
                          r=[('psf', bO), K('rec', hp)], w=[K('mixT', 'm', h)])

            emit_loads(0)
            emit_loads(1)
            for idx in range(len(items)):
                h, kt = items[idx]
                if kt == 0 and h >= 1 and h + 1 < 8:
                    emit_loads(h + 1)
                if kt == 0 and h == 1:
                    trg_emit(0)
                if kt == 0 and h == 3:
                    trg_emit(1)
                if kt == 0 and tb + 1 < NTB:
                    if h == 2:
                        pro2(0)
                        pro1(tb + 1, 1)
                    if h == 5:
                        pro2(1)
                emit_s(idx)
                if idx >= LAG:
                    emit_pv(idx - LAG)
            for idx in range(max(0, len(items) - LAG), len(items)):
                emit_pv(idx)
            pbank[0] = 0
            mixk = [K('mixT', 'g', 0), K('mixT', 'g', 128)] + [K('mixT', 'm', h) for h in range(8)]
            MS = [mst, lae]
            MK = [[K('mst', j) for j in range(4)], LAEK]
            for j in range(4):
                sl = wload(woutb, j * 512, 512)
                for s in range(2):
                    b = nb()

                    def mmo(e, b=b, s=s, sl=sl):
                        for k in range(KD):
                            ins = e.matmul(psf[b][:], lhsT=mixT[:, k, s * 128:(s + 1) * 128], rhs=wst[sl][:, k, :], start=(k == 0), stop=(k == KD - 1))
                        return ins
                    S.add('pe', mmo, r=mixk + [K('wst', sl)], w=[('psf', b)])
                    S.add('dve', lambda e, b=b, j=j, s=s: e.tensor_copy(out=MS[s][:, j * 512:(j + 1) * 512], in_=psf[b][:]),
                          r=[('psf', b)], w=([K('mst', j)] if s == 0 else LAEK))
            if tb + 1 < NTB:
                preloaded['lr'] = wload(winb, C_LR, 16)
                preloaded['gv0'] = wload(winb, C_GV, 512)
            for s in range(2):
                mk = MK[s]
                S.add('pool', lambda e, s=s: e.memset(ss[:, 8 + s:9 + s], 0.0), w=[K('ss', 8 + s)])
                S.add('act', lambda e, s=s: e.activation(out=junk[:], in_=MS[s][:], func=AF.Square, accum_out=ss[:, 8 + s:9 + s]),
                      r=mk + [K('ss', 8 + s)], w=[K('junk'), K('ss', 8 + s)])
                rstd_of(8 + s, 1, 1.0 / D)
                S.add('dve', lambda e, s=s: e.scalar_tensor_tensor(out=MS[s][:], in0=MS[s][:], scalar=rstd[:, 8 + s:9 + s], in1=gpost[:],
                                                                   op0=ALU.mult, op1=ALU.mult), r=mk + [K('rstd', 8 + s), K('gpost')], w=mk)
                S.add('dve', lambda e, s=s: e.tensor_tensor(out=hb[:, s, :], in0=MS[s][:], in1=hb[:, s, :], op=ALU.add),
                      r=mk + [K('hb', s)], w=[K('hb', s)])
                S.add('sp', lambda e, s=s: e.dma_start(out=dst[tok0 + s * 128:tok0 + (s + 1) * 128, :], in_=hb[:, s, :]),
                      r=[K('hb', s)], w=[('dram', 'Bdst', tb, s)], dma='Bo')

        for tb_ in range(NTB):
            do_tile(tb_)

    if "A" in phases:
        ffn_phase("A", x, h1 if ("B" in phases or "C" in phases) else out, "ffn1_w_gate", "ffn1_w_up", "ffn1_w_down",
                  gains["ffn1_pre_norm"], gains["ffn1_post_norm"])
        S.barrier()
    if "B" in phases:
        mixer_phase(h1 if "A" in phases else x, h2 if "C" in phases else out)
        S.barrier()
    if "C" in phases:
        srcC = h2 if "B" in phases else (h1 if "A" in phases else x)
        ffn_phase("C", srcC, out, "ffn2_w_gate", "ffn2_w_up", "ffn2_w_down",
                  gains["ffn2_pre_norm"], gains["ffn2_post_norm"])
    ok, mx = S.simulate()
    assert ok, 'sync deadlock'
    S.emit()
    return nc


NCB = 1792 + 3072


def make_consts():
    c = np.zeros((128, 128 + NCB), np.float32)
    c[:, 0:128] = np.eye(128, dtype=np.float32)
    b = c[:, 128:]
    j = np.arange(128)[:, None]
    i = np.arange(128)[None, :]
    T = (j <= i).astype(np.float32)
    b[:, 0:128] = T
    b[:, 128:256] = (j > i).astype(np.float32)
    b[:, 256:768] = np.tile(T, (1, 4))
    t = np.arange(256)[None, :]
    sl = np.arange(128)[:, None]
    b[:, 768:1024] = (t - sl).astype(np.float32)
    for kt in range(2):
        dd = (t - (kt * 128 + sl)).astype(np.float32)
        b[:, 1024 + kt * 256:1280 + kt * 256] = np.where(dd >= 0, dd, 1e6)
    slopes = 2.0 ** (-8.0 * np.arange(1, 9) / 8.0)
    for h in range(8):
        for m in range(16):
            b[:, 1536 + h * 16 + m] = -slopes[h] * m * 128.0
    for qb in range(8):
        n = np.arange(8)
        pastneg = np.where(n < qb, 0.0, -1e30).astype(np.float32)
        past01 = (n < qb).astype(np.float32)
        c3 = ((n == qb).astype(np.float32) - 1.0) * 32768.0
        base = 1664 + qb * 384
        b[:, base:base + 128] = np.tile(pastneg, 16)[None, :]
        b[:, base + 128:base + 256] = np.tile(past01, 16)[None, :]
        b[:, base + 256:base + 384] = np.tile(c3, 16)[None, :]
    return c


def make_ens():
    e = np.zeros((8, 1024), np.float32)
    for n in range(8):
        e[n, n * 128:(n + 1) * 128] = 1.0
    return e


_NC_CACHE = {}


def kernel(**inputs):
    x = np.ascontiguousarray(inputs["x"], dtype=np.float32)
    B = x.shape[0]
    per = B // NCORES
    ntok = per * SEQ
    key = ntok
    if key not in _NC_CACHE:
        _NC_CACHE[key] = build_program(ntok)
    nc = _NC_CACHE[key]
    shared = {}
    for n in ("ffn1_w_gate", "ffn1_w_up", "ffn1_w_down", "ffn2_w_gate", "ffn2_w_up", "ffn2_w_down", "w_in", "w_out"):
        shared[n] = np.ascontiguousarray(inputs[n][0], dtype=np.float32)
    for n in ("ffn1_pre_norm", "ffn1_post_norm", "mix_pre_norm", "mix_post_norm", "ffn2_pre_norm", "ffn2_post_norm",
              "gla_b_decay", "gla_out_norm"):
        shared[n] = np.ascontiguousarray(inputs[n], dtype=np.float32).reshape(1, -1)
    shared["gla_w_decay_up"] = np.ascontiguousarray(inputs["gla_w_decay_up"][0], dtype=np.float32)
    shared["consts"] = make_consts()
    shared["ens"] = make_ens()
    in_maps = []
    for c in range(NCORES):
        m = dict(shared)
        m["x"] = x[c * per:(c + 1) * per].reshape(ntok, D)
        in_maps.append(m)
    res = run_bass_kernel_spmd(nc, in_maps, core_ids=list(range(NCORES)))
    outs = [np.asarray(r["out"]).reshape(per, SEQ, D) for r in res.results]
    return np.concatenate(outs, axis=0).astype(np.float32)
```
